# Optimizing a Trainium2 kernel written in Bass

```python
import math
import jax, jax.numpy as jnp
from jax import lax
import numpy as np

D_MODEL = 1024
BATCH = 16
SEQ = 2048
DEPTH = 4

CTX_LEN = 256
GRID_W = 64
N_MIXERS = 4
EPS = 1e-6
F32 = jnp.float32

SSM_INNER = 2 * D_MODEL
SSM_HEAD_DIM = 64
SSM_HEADS = SSM_INNER // SSM_HEAD_DIM
SSM_GROUPS = 4
SSM_STATE = 128
SSM_CONV = 4
SSM_CHUNK = 128
SSM_CONV_DIM = SSM_INNER + 2 * SSM_GROUPS * SSM_STATE
SSM_IN_DIM = SSM_INNER + SSM_CONV_DIM + 2 * SSM_HEADS

ATT_HEAD_DIM = 64
ATT_Q_HEADS = D_MODEL // ATT_HEAD_DIM
ATT_KV_HEADS = 4
ATT_BLOCK = 128
ROPE_THETA = 10000.0

POOL_WINDOWS = (2, 4, 8, 16)
POOL_GROUP = D_MODEL // len(POOL_WINDOWS)

LRU_WIDTH = 1280
LRU_BLOCKS = 10
LRU_BLOCK_W = LRU_WIDTH // LRU_BLOCKS
LRU_CONV = 4
LRU_C = 8.0

FFN_HIDDEN = 3584
N_EXPERTS = 8
TOP_K = 2
MOE_BLOCK = 512

kernel_name = "hybrid_interleaved_diffusion_trunk"


def rmsnorm(u, g):
    uf = u.astype(F32)
    y = uf * lax.rsqrt(jnp.mean(uf * uf, axis=-1, keepdims=True) + EPS)
    return (y * g.astype(F32)).astype(u.dtype)


def modulate(u, g, shift, scale):
    return rmsnorm(u, g) * (1 + scale) + shift


def dwconv_centred(u, w, b):
    K, C = w.shape
    left = K // 2
    out = lax.conv_general_dilated(u, w[:, None, :].astype(u.dtype), window_strides=(1,),
                                   padding=[(left, K - 1 - left)],
                                   dimension_numbers=('NWC', 'WIO', 'NWC'), feature_group_count=C)
    return out + b


def axial_rope_tables(seq_len):
    rows = seq_len // GRID_W
    row = jnp.repeat(jnp.arange(rows), GRID_W).astype(F32)
    col = jnp.tile(jnp.arange(GRID_W), rows).astype(F32)
    n_freq = ATT_HEAD_DIM // 4
    inv_freq = ROPE_THETA ** (-jnp.arange(n_freq, dtype=F32) / n_freq)
    ang = jnp.concatenate([row[:, None] * inv_freq, col[:, None] * inv_freq], axis=-1)
    return jnp.cos(ang), jnp.sin(ang)


def apply_rope(u, cos, sin):
    shape = (1, u.shape[1]) + (1,) * (u.ndim - 3) + (cos.shape[-1],)
    cos = cos.reshape(shape)
    sin = sin.reshape(shape)
    u1, u2 = jnp.split(u.astype(F32), 2, axis=-1)
    return jnp.concatenate([u1 * cos - u2 * sin, u1 * sin + u2 * cos], axis=-1).astype(u.dtype)


def ssd_scan(xdt, la, bm, cm, h0):
    b, n, G, R, P = xdt.shape
    N = bm.shape[-1]
    Q = SSM_CHUNK
    nc = n // Q
    xc = xdt.reshape(b, nc, Q, G, R, P)
    ac = jnp.cumsum(la.reshape(b, nc, Q, G, R), axis=2)
    bc = bm.reshape(b, nc, Q, G, N)
    cc = cm.reshape(b, nc, Q, G, N)
    lower = jnp.tril(jnp.ones((Q, Q), bool))[:, :, None, None]
    decay = jnp.exp(jnp.where(lower, ac[:, :, :, None] - ac[:, :, None, :], -jnp.inf))
    cb = jnp.einsum('bclgn,bcsgn->bclsg', cc, bc)
    y_diag = jnp.einsum('bclsgr,bcsgrp->bclgrp', cb[..., None] * decay, xc)
    x_to_end = xc * jnp.exp(ac[:, :, -1:] - ac)[..., None]
    states = jnp.einsum('bcsgn,bcsgrp->bcgrpn', bc, x_to_end)
    chunk_decay = jnp.exp(ac[:, :, -1])

    def step(h, inp):
        d, s = inp
        return h * d[..., None, None] + s, h

    h_last, h_in = lax.scan(step, h0, (jnp.moveaxis(chunk_decay, 1, 0), jnp.moveaxis(states, 1, 0)))
    h_in = jnp.moveaxis(h_in, 0, 1)
    y_off = jnp.einsum('bclgn,bcgrpn->bclgrp', cc, h_in) * jnp.exp(ac)[..., None]
    return (y_diag + y_off).reshape(b, n, G, R, P), h_last


def mamba2_mixer(ul, uc, w_in, conv_w, conv_b, dt_bias, a_log, d_skip, norm_g, w_out, ctx_out):
    G, R, P, N, H = SSM_GROUPS, SSM_HEADS // SSM_GROUPS, SSM_HEAD_DIM, SSM_STATE, SSM_HEADS
    neg_a = -jnp.exp(a_log.astype(F32))

    def project(u):
        b, n, _ = u.shape
        zxbcdt = u @ w_in
        z = zxbcdt[..., :SSM_INNER]
        xbc = jax.nn.silu(dwconv_centred(zxbcdt[..., SSM_INNER:SSM_INNER + SSM_CONV_DIM], conv_w, conv_b))
        xs = xbc[..., :SSM_INNER].reshape(b, n, G, R, P).astype(F32)
        bm = xbc[..., SSM_INNER:SSM_INNER + G * N].reshape(b, n, G, N).astype(F32)
        cm = xbc[..., SSM_INNER + G * N:].reshape(b, n, G, N).astype(F32)
        dt = jax.nn.softplus(zxbcdt[..., SSM_INNER + SSM_CONV_DIM:].reshape(b, n, 2, H).astype(F32)
                             + dt_bias.astype(F32))
        return z, xs, bm, cm, dt

    def scan_dir(d, xs, bm, cm, dt, h0):
        b, n = xs.shape[:2]
        dtd = dt[:, :, d].reshape(b, n, G, R)
        la = dtd * neg_a[d].reshape(G, R)
        xdt = xs * dtd[..., None]
        if d == 1:
            xdt, la, bm, cm = (jnp.flip(t, 1) for t in (xdt, la, bm, cm))
        y, h_last = ssd_scan(xdt, la, bm, cm, h0)
        if d == 1:
            y = jnp.flip(y, 1)
        return y, h_last

    def finish(z, xs, y, dtype):
        b, n = xs.shape[:2]
        y = y + xs * d_skip.astype(F32).reshape(G, R)[:, :, None]
        y = y.reshape(b, n, SSM_INNER) * jax.nn.silu(z.astype(F32))
        return rmsnorm(y, norm_g).astype(dtype) @ w_out

    zc, xsc, bmc, cmc, dtc = project(uc)
    h0 = jnp.zeros((uc.shape[0], G, R, P, N), F32)
    yc_f, hc_f = scan_dir(0, xsc, bmc, cmc, dtc, h0)
    yc_b, hc_b = scan_dir(1, xsc, bmc, cmc, dtc, h0)
    zl, xsl, bml, cml, dtl = project(ul)
    yl_f, _ = scan_dir(0, xsl, bml, cml, dtl, hc_f)
    yl_b, _ = scan_dir(1, xsl, bml, cml, dtl, hc_b)
    yl = finish(zl, xsl, yl_f + yl_b, ul.dtype)
    yc = finish(zc, xsc, yc_f + yc_b, uc.dtype) if ctx_out else None
    return yl, yc


def attend(q, k, v):
    s = jnp.einsum('bqgrd,bkgd->bgrqk', q, k, preferred_element_type=F32) * (ATT_HEAD_DIM ** -0.5)
    p = jax.nn.softmax(s, axis=-1).astype(v.dtype)
    return jnp.einsum('bgrqk,bkgd->bqgrd', p, v)


def gqa_mixer(ul, uc, w_qkv, q_g, k_g, w_out, rope_cos, rope_sin, ctx_out):
    R = ATT_Q_HEADS // ATT_KV_HEADS
    nq = ATT_Q_HEADS * ATT_HEAD_DIM
    nk = ATT_KV_HEADS * ATT_HEAD_DIM

    def project(u):
        b, n, _ = u.shape
        qkv = u @ w_qkv
        q = rmsnorm(qkv[..., :nq].reshape(b, n, ATT_KV_HEADS, R, ATT_HEAD_DIM), q_g)
        k = rmsnorm(qkv[..., nq:nq + nk].reshape(b, n, ATT_KV_HEADS, ATT_HEAD_DIM), k_g)
        v = qkv[..., nq + nk:].reshape(b, n, ATT_KV_HEADS, ATT_HEAD_DIM)
        return q, k, v

    ql, kl, vl = project(ul)
    qc, kc, vc = project(uc)
    ql = apply_rope(ql, rope_cos, rope_sin)
    kl = apply_rope(kl, rope_cos, rope_sin)
    keys = jnp.concatenate([kc, kl], axis=1)
    vals = jnp.concatenate([vc, vl], axis=1)
    b, n, _ = ul.shape
    nb = n // ATT_BLOCK
    q_blocks = jnp.moveaxis(ql.reshape(b, nb, ATT_BLOCK, ATT_KV_HEADS, R, ATT_HEAD_DIM), 1, 0)
    o = lax.map(lambda qb: attend(qb, keys, vals), q_blocks)
    yl = jnp.moveaxis(o, 0, 1).reshape(b, n, nq) @ w_out
    yc = attend(qc, kc, vc).reshape(uc.shape[0], uc.shape[1], nq) @ w_out if ctx_out else None
    return yl, yc


def centred_pool_minus_self(v, window):
    b, n, ch = v.shape
    csum = jnp.concatenate([jnp.zeros((b, 1, ch), v.dtype), jnp.cumsum(v, axis=1)], axis=1)
    t = jnp.arange(n)
    hi = jnp.minimum(t + window // 2, n)
    lo = jnp.maximum(t - window // 2, 0)
    count = (hi - lo).astype(v.dtype)
    return (csum[:, hi] - csum[:, lo]) / count[None, :, None] - v


def pool_mixer(ul, uc, w_in, w_grp, scale, ctx_out):
    def mix(u):
        b, n, _ = u.shape
        v = (u @ w_in).astype(F32).reshape(b, n, len(POOL_WINDOWS), POOL_GROUP)
        pooled = jnp.stack([centred_pool_minus_self(v[:, :, g], w) for g, w in enumerate(POOL_WINDOWS)], axis=2)
        out = jnp.einsum('bngi,gio->bngo', pooled.astype(u.dtype), w_grp).reshape(b, n, D_MODEL)
        return out * scale

    yl = mix(ul)
    yc = mix(uc) if ctx_out else None
    return yl, yc


def linear_recurrence(a, bx, h0, reverse):
    first = -1 if reverse else 0
    bx = bx.at[:, first].add(a[:, first] * h0)

    def combine(e1, e2):
        a1, b1 = e1
        a2, b2 = e2
        return a1 * a2, a2 * b1 + b2

    _, h = lax.associative_scan(combine, (a, bx), axis=1, reverse=reverse)
    return h


def rglru_mixer(ul, uc, w_in, conv_w, conv_b, gate_w, gate_b, lam, w_out, ctx_out):
    def project(u):
        gu = u @ w_in
        xr = dwconv_centred(gu[..., LRU_WIDTH:], conv_w, conv_b).astype(F32)
        return gu[..., :LRU_WIDTH], xr

    def coeffs(xr, d):
        b, n, _ = xr.shape
        xb = xr.reshape(b, n, LRU_BLOCKS, LRU_BLOCK_W)
        pre = jnp.einsum('bnki,jkio->bnjko', xb, gate_w[d].astype(F32)).reshape(b, n, 2, LRU_WIDTH)
        gates = jax.nn.sigmoid(pre + gate_b[d].astype(F32))
        r, i = gates[:, :, 0], gates[:, :, 1]
        log_a = -LRU_C * r * jax.nn.softplus(-lam[d].astype(F32))
        bx = jnp.sqrt(-jnp.expm1(2.0 * log_a)) * (i * xr)
        return jnp.exp(log_a), bx

    gc_pre, xrc = project(uc)
    gl_pre, xrl = project(ul)
    zero = jnp.zeros((uc.shape[0], LRU_WIDTH), F32)
    hc_f = linear_recurrence(*coeffs(xrc, 0), zero, False)
    hc_b = linear_recurrence(*coeffs(xrc, 1), zero, True)
    hl_f = linear_recurrence(*coeffs(xrl, 0), hc_f[:, -1], False)
    hl_b = linear_recurrence(*coeffs(xrl, 1), hc_b[:, 0], True)
    yl = ((hl_f + hl_b).astype(ul.dtype) * jax.nn.gelu(gl_pre)) @ w_out
    yc = ((hc_f + hc_b).astype(uc.dtype) * jax.nn.gelu(gc_pre)) @ w_out if ctx_out else None
    return yl, yc


def swiglu(u, w_gate, w_up, w_down):
    return (jax.nn.silu(u @ w_gate) * (u @ w_up)) @ w_down


def moe_swiglu(u, w_router, w_gate, w_up, w_down):
    shape = u.shape
    xt = u.reshape(-1, shape[-1])
    T = xt.shape[0]
    logits = jnp.matmul(xt, w_router, preferred_element_type=F32)
    top_val, top_idx = lax.top_k(logits, TOP_K)
    gates = jax.nn.softmax(top_val, axis=-1)
    A = T * TOP_K
    e_flat = top_idx.reshape(-1)
    tok_flat = jnp.repeat(jnp.arange(T, dtype=jnp.int32), TOP_K)
    g_flat = gates.reshape(-1)
    order = jnp.argsort(e_flat)
    e_s, tok_s, g_s = e_flat[order], tok_flat[order], g_flat[order]
    counts = jnp.bincount(e_flat, length=N_EXPERTS)
    padded = (counts + MOE_BLOCK - 1) // MOE_BLOCK * MOE_BLOCK
    start = jnp.cumsum(counts) - counts
    pend = jnp.cumsum(padded)
    pstart = pend - padded
    dest = pstart[e_s] + jnp.arange(A) - start[e_s]
    n_blocks = -(-(A + N_EXPERTS * (MOE_BLOCK - 1)) // MOE_BLOCK)
    P = n_blocks * MOE_BLOCK
    buf_tok = jnp.full((P,), T, jnp.int32).at[dest].set(tok_s)
    buf_gate = jnp.zeros((P,), F32).at[dest].set(g_s)
    block_expert = jnp.minimum(jnp.searchsorted(pend, jnp.arange(n_blocks) * MOE_BLOCK, side='right'),
                               N_EXPERTS - 1)
    x_pad = jnp.concatenate([xt, jnp.zeros((1, xt.shape[1]), xt.dtype)], axis=0)
    xb = x_pad[buf_tok].reshape(n_blocks, MOE_BLOCK, -1)
    yb = lax.map(lambda a: swiglu(a[0], w_gate[a[1]], w_up[a[1]], w_down[a[1]]), (xb, block_expert))
    yb = yb.reshape(P, -1) * buf_gate[:, None].astype(yb.dtype)
    y = jax.ops.segment_sum(yb, buf_tok, num_segments=T + 1)[:T]
    return y.reshape(shape)


def setup_inputs(seed: int = 0) -> dict:
    key = jax.random.key(seed)
    keys = iter(jax.random.split(key, 48))
    D = D_MODEL
    n_ssm = len(range(0, DEPTH, N_MIXERS))
    n_att = len(range(1, DEPTH, N_MIXERS))
    n_pool = len(range(2, DEPTH, N_MIXERS))
    n_lru = len(range(3, DEPTH, N_MIXERS))
    n_dense = len(range(0, DEPTH, 2))
    n_moe = len(range(1, DEPTH, 2))

    def normal(shape, scale):
        return jax.random.normal(next(keys), shape, F32) * scale

    def gain(shape):
        return 1.0 + 0.02 * jax.random.normal(next(keys), shape, F32)

    x = normal((BATCH, SEQ, D), 1.0)
    c = normal((BATCH, D), 1.0)
    ctx = normal((BATCH, CTX_LEN, D), 1.0)
    c_ctx = normal((D,), 1.0)
    w_mod = normal((DEPTH, D, 6 * D), 0.5 * D ** -0.5)
    b_mod = normal((DEPTH, 6 * D), 0.02)
    norm_g = gain((DEPTH, 2, D))
    ssm_w_in = normal((n_ssm, D, SSM_IN_DIM), D ** -0.5)
    ssm_conv_w = normal((n_ssm, SSM_CONV, SSM_CONV_DIM), SSM_CONV ** -0.5)
    ssm_conv_b = normal((n_ssm, SSM_CONV_DIM), 0.02)
    dt0 = jnp.exp(jax.random.uniform(next(keys), (n_ssm, 2, SSM_HEADS), F32, math.log(1e-3), math.log(1e-1)))
    ssm_dt_bias = dt0 + jnp.log(-jnp.expm1(-dt0))
    ssm_a_log = jnp.log(jax.random.uniform(next(keys), (n_ssm, 2, SSM_HEADS), F32, 1.0, 16.0))
    ssm_d = gain((n_ssm, SSM_HEADS))
    ssm_norm_g = gain((n_ssm, SSM_INNER))
    ssm_w_out = normal((n_ssm, SSM_INNER, D), SSM_INNER ** -0.5)
    att_w_qkv = normal((n_att, D, (ATT_Q_HEADS + 2 * ATT_KV_HEADS) * ATT_HEAD_DIM), D ** -0.5)
    att_q_norm = gain((n_att, ATT_HEAD_DIM))
    att_k_norm = gain((n_att, ATT_HEAD_DIM))
    att_w_out = normal((n_att, ATT_Q_HEADS * ATT_HEAD_DIM, D), (ATT_Q_HEADS * ATT_HEAD_DIM) ** -0.5)
    pool_w_in = normal((n_pool, D, D), D ** -0.5)
    pool_w_grp = normal((n_pool, len(POOL_WINDOWS), POOL_GROUP, POOL_GROUP), POOL_GROUP ** -0.5)
    pool_scale = gain((n_pool, D))
    lru_w_in = normal((n_lru, D, 2 * LRU_WIDTH), D ** -0.5)
    lru_conv_w = normal((n_lru, LRU_CONV, LRU_WIDTH), LRU_CONV ** -0.5)
    lru_conv_b = normal((n_lru, LRU_WIDTH), 0.02)
    lru_gate_w = normal((n_lru, 2, 2, LRU_BLOCKS, LRU_BLOCK_W, LRU_BLOCK_W), LRU_BLOCK_W ** -0.5)
    lru_gate_b = normal((n_lru, 2, 2, LRU_WIDTH), 0.02)
    a0 = jax.random.uniform(next(keys), (n_lru, 2, LRU_WIDTH), F32, 0.9, 0.999) ** (1.0 / LRU_C)
    lru_lambda = jnp.log(a0) - jnp.log1p(-a0)
    lru_w_out = normal((n_lru, LRU_WIDTH, D), LRU_WIDTH ** -0.5)
    ffn_w_gate = normal((n_dense, D, FFN_HIDDEN), D ** -0.5)
    ffn_w_up = normal((n_dense, D, FFN_HIDDEN), D ** -0.5)
    ffn_w_down = normal((n_dense, FFN_HIDDEN, D), FFN_HIDDEN ** -0.5)
    moe_w_router = normal((n_moe, D, N_EXPERTS), D ** -0.5)
    moe_w_gate = normal((n_moe, N_EXPERTS, D, FFN_HIDDEN), D ** -0.5)
    moe_w_up = normal((n_moe, N_EXPERTS, D, FFN_HIDDEN), D ** -0.5)
    moe_w_down = normal((n_moe, N_EXPERTS, FFN_HIDDEN, D), FFN_HIDDEN ** -0.5)
    return {"x": x, "c": c, "ctx": ctx, "c_ctx": c_ctx, "w_mod": w_mod, "b_mod": b_mod, "norm_g": norm_g,
            "ssm_w_in": ssm_w_in, "ssm_conv_w": ssm_conv_w, "ssm_conv_b": ssm_conv_b,
            "ssm_dt_bias": ssm_dt_bias, "ssm_a_log": ssm_a_log, "ssm_d": ssm_d, "ssm_norm_g": ssm_norm_g,
            "ssm_w_out": ssm_w_out, "att_w_qkv": att_w_qkv, "att_q_norm": att_q_norm,
            "att_k_norm": att_k_norm, "att_w_out": att_w_out, "pool_w_in": pool_w_in,
            "pool_w_grp": pool_w_grp, "pool_scale": pool_scale, "lru_w_in": lru_w_in,
            "lru_conv_w": lru_conv_w, "lru_conv_b": lru_conv_b, "lru_gate_w": lru_gate_w,
            "lru_gate_b": lru_gate_b, "lru_lambda": lru_lambda, "lru_w_out": lru_w_out,
            "ffn_w_gate": ffn_w_gate, "ffn_w_up": ffn_w_up, "ffn_w_down": ffn_w_down,
            "moe_w_router": moe_w_router, "moe_w_gate": moe_w_gate, "moe_w_up": moe_w_up,
            "moe_w_down": moe_w_down}


def reference(x, c, ctx, c_ctx, w_mod, b_mod, norm_g, ssm_w_in, ssm_conv_w, ssm_conv_b, ssm_dt_bias,
              ssm_a_log, ssm_d, ssm_norm_g, ssm_w_out, att_w_qkv, att_q_norm, att_k_norm, att_w_out,
              pool_w_in, pool_w_grp, pool_scale, lru_w_in, lru_conv_w, lru_conv_b, lru_gate_w, lru_gate_b,
              lru_lambda, lru_w_out, ffn_w_gate, ffn_w_up, ffn_w_down, moe_w_router, moe_w_gate, moe_w_up,
              moe_w_down):
    rope_cos, rope_sin = axial_rope_tables(x.shape[1])
    s_lat = jax.nn.silu(c)
    s_ctx = jax.nn.silu(c_ctx)
    h, hc = x, ctx
    for i in range(DEPTH):
        last = i == DEPTH - 1
        mod_l = jnp.split((s_lat @ w_mod[i] + b_mod[i])[:, None, :], 6, axis=-1)
        mod_c = jnp.split((s_ctx @ w_mod[i] + b_mod[i])[None, None, :], 6, axis=-1)
        ul = modulate(h, norm_g[i, 0], mod_l[0], mod_l[1])
        uc = modulate(hc, norm_g[i, 0], mod_c[0], mod_c[1])
        kind, j = i % N_MIXERS, i // N_MIXERS
        if kind == 0:
            yl, yc = mamba2_mixer(ul, uc, ssm_w_in[j], ssm_conv_w[j], ssm_conv_b[j], ssm_dt_bias[j],
                                  ssm_a_log[j], ssm_d[j], ssm_norm_g[j], ssm_w_out[j], not last)
        elif kind == 1:
            yl, yc = gqa_mixer(ul, uc, att_w_qkv[j], att_q_norm[j], att_k_norm[j], att_w_out[j],
                               rope_cos, rope_sin, not last)
        elif kind == 2:
            yl, yc = pool_mixer(ul, uc, pool_w_in[j], pool_w_grp[j], pool_scale[j], not last)
        else:
            yl, yc = rglru_mixer(ul, uc, lru_w_in[j], lru_conv_w[j], lru_conv_b[j], lru_gate_w[j],
                                 lru_gate_b[j], lru_lambda[j], lru_w_out[j], not last)
        h = h + mod_l[2] * yl
        if not last:
            hc = hc + mod_c[2] * yc
        if i % 2 == 0:
            k = i // 2
            ffn = lambda u: swiglu(u, ffn_w_gate[k], ffn_w_up[k], ffn_w_down[k])
        else:
            k = i // 2
            ffn = lambda u: moe_swiglu(u, moe_w_router[k], moe_w_gate[k], moe_w_up[k], moe_w_down[k])
        h = h + mod_l[5] * ffn(modulate(h, norm_g[i, 1], mod_l[3], mod_l[4]))
        if not last:
            hc = hc + mod_c[5] * ffn(modulate(hc, norm_g[i, 1], mod_c[3], mod_c[4]))
    return h
```

```python
import numpy as np
from contextlib import ExitStack
import concourse.bass as bass
import concourse.mybir as mybir
from concourse.bass_utils import run_bass_kernel_spmd

F32 = mybir.dt.float32
BF16 = mybir.dt.bfloat16
I32 = mybir.dt.int32
AF = mybir.ActivationFunctionType
ALU = mybir.AluOpType
AX = mybir.AxisListType

D = 1024
NB = 2
CTX = 256
SEQ = 2048
TOK = CTX + SEQ
EPS = 1e-6
FFN = 3584
NE = 8
NDS = 24


class Dep:
    __slots__ = ("w", "r")

    def __init__(self):
        self.w = None
        self.r = {}


class T:
    def __init__(self, ap, dep=None):
        self.ap = ap
        self.dep = dep if dep is not None else Dep()


def _deps_of(lst):
    out = []
    for x in lst:
        if isinstance(x, T):
            out.append(x.dep)
        elif isinstance(x, Dep):
            out.append(x)
        else:
            out.extend(_deps_of(x))
    return out


class Prog:
    def __init__(self, nc, es):
        self.nc = nc
        self.eng = {"pe": nc.tensor, "act": nc.scalar, "dve": nc.vector, "pool": nc.gpsimd, "sp": nc.sync}
        self.sem = {}
        self.cnt = {}
        self.waited = {k: {} for k in self.eng}
        for k in self.eng:
            self.sem[k] = es.enter_context(nc.semaphore("s_" + k))
            self.cnt[k] = 0
        self.dq = {}
        self.dqi = {}
        for q in ("sp", "act", "pool"):
            self.dq[q] = []
            self.dqi[q] = 0
            for i in range(NDS):
                key = ("d", q, i)
                self.sem[key] = es.enter_context(nc.semaphore(f"d_{q}{i}"))
                self.cnt[key] = 0
                self.dq[q].append(key)
        self.ps = [T(es.enter_context(nc.psum_tensor(f"ps{i}", [128, 512], F32))[:]) for i in range(8)]
        self.psi = {"A": 0, "B": 0, "C": 0}
        self.n_ins = 0

    def psA(self):
        self.psi["A"] += 1
        return self.ps[self.psi["A"] % 3]

    def psB(self):
        self.psi["B"] += 1
        return self.ps[4 + self.psi["B"] % 2]

    def psC(self):
        self.psi["C"] += 1
        return self.ps[6 + self.psi["C"] % 2]

    def sb(self, es, name, shape, dtype):
        self.uid = getattr(self, "uid", 0) + 1
        return T(es.enter_context(self.nc.sbuf_tensor(f"{name}_{self.uid}", list(shape), dtype))[:])

    def _wait(self, e, key, val):
        w = self.waited[e]
        if w.get(key, 0) >= val:
            return
        w[key] = val
        self.eng[e].wait_ge(self.sem[key], val)
        self.n_ins += 1

    def _deps(self, e, reads, writes, is_dma):
        deps = {}

        def add(k, v):
            if deps.get(k, 0) < v:
                deps[k] = v

        for d in reads:
            if d.w is not None:
                if not (e == "pe" and d.w[0] == "pe" and not is_dma):
                    add(*d.w)
        for d in writes:
            if d.w is not None and (is_dma or d.w[0] != e):
                add(*d.w)
            for k, v in d.r.items():
                if is_dma or k != e:
                    add(k, v)
        return deps

    def op(self, e, fn, reads=(), writes=(), signal=True):
        reads = _deps_of(reads)
        writes = _deps_of(writes)
        for k, v in self._deps(e, reads, writes, False).items():
            self._wait(e, k, v)
        ins = fn(self.eng[e])
        self.n_ins += 1
        if signal:
            self.cnt[e] += 1
            ins.then_inc(self.sem[e], 1)
            val = self.cnt[e]
        else:
            val = self.cnt[e] + 1
        for d in reads:
            if d.r.get(e, 0) < val:
                d.r[e] = val
        for d in writes:
            d.w = (e, val)
            d.r = {}
        return ins

    def dma(self, q, out, in_, reads=(), writes=(), **kw):
        reads = _deps_of(reads)
        writes = _deps_of(writes)
        key = self.dq[q][self.dqi[q] % NDS]
        self.dqi[q] += 1
        deps = self._deps(q, reads, writes, True)
        if self.cnt[key] > 0 and deps.get(key, 0) < self.cnt[key]:
            deps[key] = self.cnt[key]
        for k, v in deps.items():
            self._wait(q, k, v)
        ins = self.eng[q].dma_start(out=out, in_=in_, **kw)
        self.n_ins += 1
        self.cnt[key] += 16
        ins.then_inc(self.sem[key], 16)
        val = self.cnt[key]
        for d in reads:
            d.r[key] = val
        for d in writes:
            d.w = (key, val)
            d.r = {}

    def barrier(self):
        snap = dict(self.cnt)
        for e in self.eng:
            for k_, v in snap.items():
                if k_ != e and v > 0:
                    self._wait(e, k_, v)

    def finish(self):
        self.barrier()

    def mm(self, ps, out, lhsT, rhs, reads, start, stop):
        if lhsT.dtype == F32 and stop:
            self.op("pe", lambda e: e.matmul(out=out, lhsT=lhsT, rhs=rhs, start=start, stop=stop),
                    reads=reads, writes=[ps], signal=False)
            fb = self.ps[3]
            self.op("pe", lambda e: e.matmul(out=fb.ap[0:1, 0:2], lhsT=self.fence_src[0:1, 0:1],
                                             rhs=self.fence_src[0:1, 0:2], start=True, stop=True),
                    reads=[], writes=[ps], signal=True)
            return
        self.op("pe", lambda e: e.matmul(out=out, lhsT=lhsT, rhs=rhs, start=start, stop=stop),
                reads=reads, writes=[ps], signal=stop)

    def tr(self, ps, out, in_, ident, reads, signal=True, np_=128):
        self.op("pe", lambda e: e.transpose(out=out, in_=in_, identity=ident.ap[0:np_, 0:np_]),
                reads=list(reads) + [ident], writes=[ps], signal=signal)

    def act(self, out, in_, func, reads, writes, bias=None, scale=None, eng="act"):
        kw = {}
        if bias is not None:
            kw["bias"] = bias
        if scale is not None:
            kw["scale"] = scale
        self.op("act", lambda e: e.activation(out=out, in_=in_, func=func, **kw), reads=reads, writes=writes)

    def tt(self, e, out, in0, in1, op, reads, writes):
        self.op(e, lambda g: g.tensor_tensor(out=out, in0=in0, in1=in1, op=op), reads=reads, writes=writes)

    def ts(self, e, out, in0, s1, s2, op0, op1, reads, writes):
        if op1 is None:
            self.op(e, lambda g: g.tensor_scalar(out=out, in0=in0, scalar1=s1, scalar2=None, op0=op0),
                    reads=reads, writes=writes)
        else:
            self.op(e, lambda g: g.tensor_scalar(out=out, in0=in0, scalar1=s1, scalar2=s2, op0=op0, op1=op1),
                    reads=reads, writes=writes)

    def stt(self, out, in0, scalar, in1, op0, op1, reads, writes):
        self.op("dve", lambda g: g.scalar_tensor_tensor(out=out, in0=in0, scalar=scalar, in1=in1, op0=op0, op1=op1),
                reads=reads, writes=writes)

    def copy(self, e, out, in_, reads, writes):
        if e == "act":
            self.op("act", lambda g: g.copy(out=out, in_=in_), reads=reads, writes=writes)
        else:
            self.op(e, lambda g: g.tensor_copy(out=out, in_=in_), reads=reads, writes=writes)

    def memset(self, e, t, ap, val):
        self.op(e, lambda g: g.memset(ap, val), reads=[], writes=[t])


WEIGHT_SPECS = [
    ("c_ctx", [1024]), ("w_mod", [4, 1024, 6144]), ("b_mod", [4, 6144]), ("norm_g", [4, 2, 1024]),
    ("ssm_w_in", [1, 1024, 5184]), ("ssm_conv_w", [1, 4, 3072]), ("ssm_conv_b", [1, 3072]),
    ("ssm_dt_bias", [1, 2, 32]), ("ssm_a_log", [1, 2, 32]), ("ssm_d", [1, 32]), ("ssm_norm_g", [1, 2048]),
    ("ssm_w_out", [1, 2048, 1024]), ("att_w_qkv", [1, 1024, 1536]), ("att_q_norm", [1, 64]),
    ("att_k_norm", [1, 64]), ("att_w_out", [1, 1024, 1024]), ("pool_w_in", [1, 1024, 1024]),
    ("pool_w_grp", [1, 4, 256, 256]), ("pool_scale", [1, 1024]), ("lru_w_in", [1, 1024, 2560]),
    ("lru_conv_w", [1, 4, 1280]), ("lru_conv_b", [1, 1280]), ("lru_gate_w", [1, 2, 2, 10, 128, 128]),
    ("lru_gate_b", [1, 2, 2, 1280]), ("lru_lambda", [1, 2, 1280]), ("lru_w_out", [1, 1280, 1024]),
    ("ffn_w_gate", [2, 1024, 3584]), ("ffn_w_up", [2, 1024, 3584]), ("ffn_w_down", [2, 3584, 1024]),
    ("moe_w_router", [2, 1024, 8]), ("moe_w_gate", [2, 8, 1024, 3584]), ("moe_w_up", [2, 8, 1024, 3584]),
    ("moe_w_down", [2, 8, 3584, 1024]),
]


def token_tiles(b_list=(0, 1), with_ctx=True, width=512):
    out = []
    for b in b_list:
        if with_ctx:
            out.append((b, 2, 0, CTX))
        for t0 in range(0, SEQ, width):
            out.append((b, b, CTX + t0, min(width, SEQ - t0)))
    return out


class K:
    pass


def hdeps(arr, b, t0, n):
    return [arr[b][j] for j in range(t0 // 128, (t0 + n + 127) // 128)]


def build(stages=None, debug=False):
    nc = bass.Bass("TRN2", target_bir_lowering=False)
    k = K()
    k.nc = nc
    k.stages = stages
    import os
    k.dbg_barrier = bool(os.environ.get("DBG_BARRIER"))
    k.pool_eng = os.environ.get("POOL_ENG", "dve")
    dt = nc.dram_tensor
    k.x = dt("x", [NB, SEQ, D], F32, kind="ExternalInput").ap()
    k.c = dt("c", [NB, D], F32, kind="ExternalInput").ap()
    k.ctx = dt("ctx", [NB, CTX, D], F32, kind="ExternalInput").ap()
    k.w = {}
    k.dummy = set()
    pref = {"mix0": "ssm_", "mix1": "att_", "mix2": "pool_", "mix3": "lru_", "ffn0": "ffn_", "ffn2": "ffn_",
            "ffn1": "moe_", "ffn3": "moe_"}
    for name, shape in WEIGHT_SPECS:
        if stages is not None and name.split("_")[0] in ("ssm", "att", "pool", "lru", "ffn", "moe") and \
                not any(name.startswith(pref[s_]) for s_ in stages):
            k.dummy.add(name)
            shape = [128]
        k.w[name] = dt(name, shape, F32, kind="ExternalInput").ap()
    k.rope_cos = dt("rope_cos", [64, SEQ], F32, kind="ExternalInput").ap()
    k.rope_sin = dt("rope_sin", [64, SEQ], F32, kind="ExternalInput").ap()
    k.out = dt("out", [NB, SEQ, D], F32, kind="ExternalOutput").ap()
    k.hT = dt("hT", [NB, D, TOK], F32, kind="ExternalOutput" if debug else "Internal").ap()
    k.uT = dt("uT", [NB, D, TOK], BF16, kind="Internal").ap()
    k.aT = dt("aT", [NB, 2048, TOK], BF16, kind="Internal").ap()
    k.yS = dt("yS", [NB, TOK, 2048], BF16, kind="ExternalOutput" if debug else "Internal").ap()
    if debug:
        k.dbg = dt("dbg", [128, 576], F32, kind="ExternalOutput").ap()
        k.dbg2 = dt("dbg2", [4, 128, 1024], BF16, kind="ExternalOutput").ap()
        k.dbg3 = dt("dbg3", [2, 128, 9216], F32, kind="ExternalOutput").ap()
        k.dbgd = Dep()
    k.debug = debug
    k.hTd = [[Dep() for _ in range(18)] for _ in range(NB)]
    k.uTd = [[Dep() for _ in range(18)] for _ in range(NB)]
    k.aTd = [[Dep() for _ in range(18)] for _ in range(NB)]
    k.ySd = [[Dep() for _ in range(18)] for _ in range(NB)]
    k.outd = Dep()

    with ExitStack() as es:
        P = Prog(nc, es)
        k.P = P
        emit_all(k, es)
        P.finish()
    k.n_ins = P.n_ins
    return nc, k


def emit_consts(k, es):
    P = k.P
    k.identF = P.sb(es, "identF", [128, 128], F32)
    k.identB = P.sb(es, "identB", [128, 128], BF16)
    k.onesF = P.sb(es, "onesF", [128, 128], F32)
    k.onesB = P.sb(es, "onesB", [128, 128], BF16)
    P.memset("dve", k.onesF, k.onesF.ap, 1.0)
    P.memset("dve", k.onesB, k.onesB.ap, 1.0)
    P.fence_src = k.onesB.ap

    def sel(dst, pattern_step, base, cm, op=ALU.is_ge, src=None):
        src = src or k.onesF
        P.op("pool", lambda g: g.affine_select(out=dst.ap, in_=src.ap, pattern=[[pattern_step, 128]], compare_op=op,
                                               fill=0.0, base=base, channel_multiplier=cm),
             reads=[src], writes=[dst])

    sel(k.identF, 1, 0, -1, ALU.is_equal)
    P.copy("dve", k.identB.ap, k.identF.ap, [k.identF], [k.identB])
    k.m_le = P.sb(es, "m_le", [128, 128], F32)
    k.m_ge = P.sb(es, "m_ge", [128, 128], F32)
    k.m_gt = P.sb(es, "m_gt", [128, 128], F32)
    k.m_lt = P.sb(es, "m_lt", [128, 128], F32)
    sel(k.m_le, 1, 0, -1)
    sel(k.m_ge, -1, 0, 1)
    sel(k.m_gt, -1, -1, 1)
    sel(k.m_lt, 1, -1, -1)


def colvec(k, dst, dst_ap, src, src_ap, C, rows=128):
    P = k.P
    ps = P.psC()
    P.tr(ps, ps.ap[0:rows, 0:C], src_ap, k.identF, [src], np_=C)
    P.copy("dve", dst_ap, ps.ap[0:rows, 0:C], [ps], [dst])


def emit_load(k):
    P = k.P
    with ExitStack() as es:
        xin = [P.sb(es, f"xin{i}", [128, 4, D], F32) for i in range(2)]
        stg = [P.sb(es, f"stg{i}", [128, 8, 512], F32) for i in range(2)]
        it = 0
        for b in range(NB):
            for (src, tok0, nblk) in ((k.ctx, 0, CTX // 128), (k.x, CTX, SEQ // 128)):
                for g0 in range(0, nblk, 4):
                    nb = min(4, nblk - g0)
                    xi, sg = xin[it % 2], stg[it % 2]
                    it += 1
                    P.dma("sp", xi.ap[:, 0:nb, :],
                          src[b, g0 * 128:(g0 + nb) * 128, :].rearrange("(j p) f -> p j f", p=128), [], [xi])
                    for c in range(8):
                        ps = P.psA()
                        for j in range(nb):
                            P.tr(ps, ps.ap[:, j * 128:(j + 1) * 128], xi.ap[:, j, c * 128:(c + 1) * 128], k.identF,
                                 [xi], signal=(j == nb - 1))
                        P.copy("act" if c % 2 == 0 else "dve", sg.ap[:, c, 0:nb * 128], ps.ap[:, 0:nb * 128], [ps], [sg])
                    t0 = tok0 + g0 * 128
                    P.dma("act", k.hT[b, :, t0:t0 + nb * 128].rearrange("(c p) t -> p c t", p=128),
                          sg.ap[:, :, 0:nb * 128], [sg], hdeps(k.hTd, b, t0, nb * 128))
    P.barrier()


def emit_store(k):
    P = k.P
    with ExitStack() as es:
        hin = [P.sb(es, f"hin{i}", [128, 8, 512], F32) for i in range(2)]
        ost = [P.sb(es, f"ost{i}", [128, D], F32) for i in range(2)]
        it = 0
        io = 0
        for b in range(NB):
            for t0 in range(0, SEQ, 512):
                hi = hin[it % 2]
                it += 1
                P.dma("sp", hi.ap, k.hT[b, :, CTX + t0:CTX + t0 + 512].rearrange("(c p) t -> p c t", p=128),
                      hdeps(k.hTd, b, CTX + t0, 512), [hi])
                for j in range(4):
                    os_ = ost[io % 2]
                    io += 1
                    for half in range(2):
                        ps = P.psA()
                        for cc in range(4):
                            c = half * 4 + cc
                            P.tr(ps, ps.ap[:, cc * 128:(cc + 1) * 128], hi.ap[:, c, j * 128:(j + 1) * 128], k.identF,
                                 [hi], signal=(cc == 3))
                        P.copy("act" if half == 0 else "dve", os_.ap[:, half * 512:(half + 1) * 512], ps.ap, [ps], [os_])
                    P.dma("act", k.out[b, t0 + j * 128:t0 + (j + 1) * 128, :], os_.ap, [os_], [k.outd])
    P.barrier()


def emit_mod(k, es):
    P = k.P
    k.modT = P.sb(es, "modT", [128, 4, 48, 3], F32)
    k.gT = P.sb(es, "gT", [128, 4, 2, 8], F32)
    k.A = P.sb(es, "Amod", [128, 4, 2, 8, 3], F32)
    with ExitStack() as es2:
        sT = P.sb(es2, "sT", [128, 8, 3], F32)
        bm = P.sb(es2, "bm", [128, 4, 48], F32)
        wsl = [P.sb(es2, f"wsl{i}", [128, 8, 512], F32) for i in range(2)]
        cr = P.sb(es2, "cr", [8, 3, 128], F32)
        for b in range(NB):
            P.dma("sp", cr.ap[:, b, :], k.c[b, :].rearrange("(c p) -> c p", p=128), [], [cr])
        P.dma("sp", cr.ap[:, 2, :], k.w["c_ctx"].rearrange("(c p) -> c p", p=128), [], [cr])
        for r in range(3):
            colvec(k, sT, sT.ap[:, :, r], cr, cr.ap[:, r, :], 8)
        bmr = P.sb(es2, "bmr", [48, 4, 128], F32)
        P.dma("sp", bmr.ap, k.w["b_mod"].rearrange("i (j p) -> j i p", p=128), [], [bmr])
        gr = P.sb(es2, "gr", [8, 8, 128], F32)
        P.dma("sp", gr.ap, k.w["norm_g"].rearrange("i w (c p) -> c (i w) p", p=128), [], [gr])
        for i in range(4):
            colvec(k, bm, bm.ap[:, i, :], bmr, bmr.ap[:, i, :], 48)
            for w in range(2):
                colvec(k, k.gT, k.gT.ap[:, i, w, :], gr, gr.ap[:, i * 2 + w, :], 8)
        P.act(sT.ap, sT.ap, AF.Silu, [sT], [sT])
        it = 0
        for i in range(4):
            for s in range(12):
                ws = wsl[it % 2]
                it += 1
                P.dma("sp" if it % 2 == 0 else "act", ws.ap,
                      k.w["w_mod"][i, :, s * 512:(s + 1) * 512].rearrange("(c p) f -> p c f", p=128), [], [ws])
                ps = P.psC()
                for cc in range(4):
                    for kc in range(8):
                        P.mm(ps, ps.ap[:, cc * 3:(cc + 1) * 3], ws.ap[:, kc, cc * 128:(cc + 1) * 128], sT.ap[:, kc, :],
                             [ws, sT], kc == 0, kc == 7)
                P.tt("dve", k.modT.ap[:, i, s * 4:(s + 1) * 4, :],
                     ps.ap[:, 0:12].rearrange("p (a b) -> p a b", b=3),
                     bm.ap[:, i, s * 4:(s + 1) * 4].unsqueeze(2).to_broadcast([128, 4, 3]), ALU.add,
                     [ps, bm], [k.modT])
        for i in range(4):
            for w in range(2):
                kind = 1 + 3 * w
                P.op("dve", lambda g: g.scalar_tensor_tensor(
                    out=k.A.ap[:, i, w, :, :], in0=k.modT.ap[:, i, kind * 8:(kind + 1) * 8, :], scalar=1.0,
                    in1=k.gT.ap[:, i, w, :].unsqueeze(2).to_broadcast([128, 8, 3]), op0=ALU.add, op1=ALU.mult),
                    reads=[k.modT, k.gT], writes=[k.A])
    P.barrier()


def modulate_tile(k, i, w, r, h, n, sq, rs, u, u32=None):
    P = k.P
    for c in range(8):
        P.act(sq.ap[:, c, 0:n], h.ap[:, c, 0:n], AF.Square, [h], [sq])
    ps = P.psC()
    for c in range(8):
        P.mm(ps, ps.ap[:, 0:n], k.onesF.ap, sq.ap[:, c, 0:n], [k.onesF, sq], c == 0, c == 7)
    P.act(rs.ap[:, 0:n], ps.ap[:, 0:n], AF.Sqrt, [ps], [rs], bias=k.epsT.ap[:, 0:1], scale=1.0 / D)
    P.op("dve", lambda g: g.reciprocal(out=rs.ap[:, 0:n], in_=rs.ap[:, 0:n]), [rs], [rs])
    shift = (3 * w) * 8
    for c in range(8):
        P.tt("dve", sq.ap[:, c, 0:n], h.ap[:, c, 0:n], rs.ap[:, 0:n], ALU.mult, [h, rs], [sq])
        dst = u32 if u32 is not None else u
        P.ts("dve" if u32 is not None else "pool", dst.ap[:, c, 0:n], sq.ap[:, c, 0:n], k.A.ap[:, i, w, c, r:r + 1],
             k.modT.ap[:, i, shift + c, r:r + 1], ALU.mult, ALU.add, [sq, k.A, k.modT], [dst])
        if u32 is not None:
            P.copy("act", u.ap[:, c, 0:n], u32.ap[:, c, 0:n], [u32], [u])


def emit_modulate(k, i, last):
    P = k.P
    with ExitStack() as es:
        hb = [P.sb(es, f"mh{j}", [128, 8, 512], F32) for j in range(2)]
        sq = P.sb(es, "msq", [128, 8, 512], F32)
        rs = P.sb(es, "mrs", [128, 512], F32)
        ub = [P.sb(es, f"mu{j}", [128, 8, 512], BF16) for j in range(2)]
        for it, (b, r, t0, n) in enumerate(token_tiles()):
            h, u = hb[it % 2], ub[it % 2]
            P.dma("sp", h.ap[:, :, 0:n], k.hT[b, :, t0:t0 + n].rearrange("(c p) t -> p c t", p=128),
                  hdeps(k.hTd, b, t0, n), [h])
            modulate_tile(k, i, 0, r, h, n, sq, rs, u)
            P.dma("act", k.uT[b, :, t0:t0 + n].rearrange("(c p) t -> p c t", p=128), u.ap[:, :, 0:n], [u],
                  hdeps(k.uTd, b, t0, n))
    P.barrier()


def load_w_bf16(k, dst, dst_ap, src_ap):
    k.P.dma("pool", dst_ap, src_ap, [], [dst])


def emit_outproj(k, i, w_ap, KC, last, gscale=None):
    P = k.P
    with ExitStack() as es:
        wo = P.sb(es, "wo", [128, KC, D], BF16)
        for kc in range(0, KC, 4):
            n = min(4, KC - kc)
            load_w_bf16(k, wo, wo.ap[:, kc:kc + n, :], w_ap[kc * 128:(kc + n) * 128, :].rearrange("(c p) f -> p c f", p=128))
        ab = [P.sb(es, f"oa{j}", [128, KC, 512], BF16) for j in range(2)]
        hb = [P.sb(es, f"oh{j}", [128, 8, 512], F32) for j in range(2)]
        for it, (b, r, t0, n) in enumerate(token_tiles(with_ctx=not last)):
            a, h = ab[it % 2], hb[it % 2]
            P.dma("sp", a.ap[:, :, 0:n], k.aT[b, 0:KC * 128, t0:t0 + n].rearrange("(c p) t -> p c t", p=128),
                  hdeps(k.aTd, b, t0, n), [a])
            P.dma("sp", h.ap[:, :, 0:n], k.hT[b, :, t0:t0 + n].rearrange("(c p) t -> p c t", p=128),
                  hdeps(k.hTd, b, t0, n), [h])
            for oc in range(8):
                ps = P.psA()
                for kc in range(KC):
                    P.mm(ps, ps.ap[:, 0:n], wo.ap[:, kc, oc * 128:(oc + 1) * 128], a.ap[:, kc, 0:n], [wo, a],
                         kc == 0, kc == KC - 1)
                P.stt(h.ap[:, oc, 0:n], ps.ap[:, 0:n], k.modT.ap[:, i, 16 + oc, r:r + 1], h.ap[:, oc, 0:n],
                      ALU.mult, ALU.add, [ps, k.modT, h], [h])
            P.dma("act", k.hT[b, :, t0:t0 + n].rearrange("(c p) t -> p c t", p=128), h.ap[:, :, 0:n], [h],
                  hdeps(k.hTd, b, t0, n))
    P.barrier()


def super_tiles(last, width=768):
    groups = []
    for b in range(NB):
        tl = token_tiles((b,), with_ctx=not last, width=256)
        cur, curw = [], 0
        for t in tl:
            if curw + t[3] > width:
                groups.append(cur)
                cur, curw = [], 0
            cur.append(t)
            curw += t[3]
        if cur:
            groups.append(cur)
    out = []
    for g in groups:
        m = []
        for (b, r, t0, n) in g:
            if m and m[-1][1] == r and m[-1][2] + m[-1][3] == t0 and m[-1][3] + n <= 512:
                m[-1] = (b, r, m[-1][2], m[-1][3] + n)
            else:
                m.append((b, r, t0, n))
        out.append(m)
    return out


def emit_ffn(k, i, last, moe):
    P = k.P
    kk = i // 2
    W = 768
    PF = 2
    with ExitStack() as es:
        hres = P.sb(es, "f_h", [128, 8, W], F32)
        u = P.sb(es, "f_u", [128, 8, W], BF16)
        hid = P.sb(es, "f_hid", [128, 28, W], BF16)
        ring = [P.sb(es, f"f_w{j}", [128, 8192], BF16) for j in range(4)]
        sq = P.sb(es, "f_sq", [128, 8, 512], F32)
        u32 = P.sb(es, "f_u32", [128, 8, 512], F32) if moe else None
        rs = P.sb(es, "f_rs", [128, 512], F32)
        sg = [P.sb(es, f"f_sg{j}", [128, 512], F32) for j in range(2)]
        if moe:
            gbc = P.sb(es, "f_gbc", [128, 8, W], BF16)
            t2 = [P.sb(es, f"f_t2{j}", [128, 512], F32) for j in range(2)]
            wr = P.sb(es, "f_wr", [128, 8, 8], F32)
            P.dma("sp", wr.ap, k.w["moe_w_router"][kk].rearrange("(c p) e -> p c e", p=128), [], [wr])
            lg = P.sb(es, "f_lg", [128, 8], F32)
            mx = P.sb(es, "f_mx", [128, 8], F32)
            sm = P.sb(es, "f_sm", [128, 8], F32)
            ex = P.sb(es, "f_ex", [128, 8], F32)
            gt = P.sb(es, "f_gt", [128, 8], F32)
            gb = P.sb(es, "f_gb", [128, 8, 128], F32)
        groups = super_tiles(last, W)
        nexp = NE if moe else 1
        work = []
        for gi in range(len(groups)):
            for e in range(nexp):
                for j in range(7):
                    work.append((gi, e, "gu", j))
                for ds in range(4):
                    work.append((gi, e, "dn", ds))

        def wts(e):
            if moe:
                return k.w["moe_w_gate"][kk, e], k.w["moe_w_up"][kk, e], k.w["moe_w_down"][kk, e]
            return k.w["ffn_w_gate"][kk], k.w["ffn_w_up"][kk], k.w["ffn_w_down"][kk]

        def issue_load(wi):
            gi, e, kind, idx = work[wi]
            slot = ring[wi % 4]
            wg_, wu_, wd_ = wts(e)
            if kind == "gu":
                sv = slot.ap.rearrange("p (g c f) -> p g c f", g=2, c=8)
                load_w_bf16(k, slot, sv[:, 0], wg_[:, idx * 512:(idx + 1) * 512].rearrange("(c p) f -> p c f", p=128))
                load_w_bf16(k, slot, sv[:, 1], wu_[:, idx * 512:(idx + 1) * 512].rearrange("(c p) f -> p c f", p=128))
            else:
                sv = slot.ap[:, 0:28 * 256].rearrange("p (c f) -> p c f", c=28)
                for q4 in range(0, 28, 7):
                    load_w_bf16(k, slot, sv[:, q4:q4 + 7, :],
                                wd_[q4 * 128:(q4 + 7) * 128, idx * 256:(idx + 1) * 256].rearrange("(c p) f -> p c f", p=128))

        def prologue(grp, offs, b):
            for (_, r, t0, n), o in zip(grp, offs):
                hv = T(hres.ap[:, :, o:o + n], hres.dep)
                uv = T(u.ap[:, :, o:o + n], u.dep)
                P.dma("sp", hv.ap, k.hT[b, :, t0:t0 + n].rearrange("(c p) t -> p c t", p=128),
                      hdeps(k.hTd, b, t0, n), [hres])
                modulate_tile(k, i, 1, r, hv, n, sq, rs, uv, u32)
                if not moe:
                    continue
                for j in range(n // 128):
                    ps = P.psC()
                    for c in range(8):
                        P.mm(ps, ps.ap[:, 0:8], u32.ap[:, c, j * 128:(j + 1) * 128], wr.ap[:, c, :], [u32, wr],
                             c == 0, c == 7)
                    P.copy("dve", lg.ap, ps.ap[:, 0:8], [ps], [lg])
                    P.op("dve", lambda g: g.max(out=mx.ap, in_=lg.ap), [lg], [mx])
                    P.ts("dve", sm.ap, lg.ap, mx.ap[:, 1:2], None, ALU.is_ge, None, [lg, mx], [sm])
                    P.ts("dve", mx.ap[:, 2:3], mx.ap[:, 0:1], -1.0, None, ALU.mult, None, [mx], [mx])
                    P.act(ex.ap, lg.ap, AF.Exp, [lg, mx], [ex], bias=mx.ap[:, 2:3], scale=1.0)
                    P.act(mx.ap[:, 3:4], mx.ap[:, 1:2], AF.Exp, [mx], [mx], bias=mx.ap[:, 2:3], scale=1.0)
                    P.ts("dve", mx.ap[:, 3:4], mx.ap[:, 3:4], 1.0, None, ALU.add, None, [mx], [mx])
                    P.op("dve", lambda g: g.reciprocal(out=mx.ap[:, 4:5], in_=mx.ap[:, 3:4]), [mx], [mx])
                    P.stt(gt.ap, ex.ap, mx.ap[:, 4:5], sm.ap, ALU.mult, ALU.mult, [ex, mx, sm], [gt])
                    P.copy("dve", gb.ap, gt.ap.unsqueeze(2).to_broadcast([128, 8, 128]), [gt], [gb])
                    for e2 in range(0, 8, 4):
                        ps2 = P.psC()
                        for e in range(e2, e2 + 4):
                            P.mm(ps2, ps2.ap[:, (e - e2) * 128:(e - e2 + 1) * 128], gb.ap[:, e, :], k.identF.ap,
                                 [gb, k.identF], True, True)
                        P.copy("act", gbc.ap[:, e2:e2 + 4, o + j * 128:o + (j + 1) * 128],
                               ps2.ap.rearrange("p (e t) -> p e t", t=128), [ps2], [gbc])

        isg = 0
        for wi in range(min(PF, len(work))):
            issue_load(wi)
        for wi, (gi, e, kind, idx) in enumerate(work):
            if wi + PF < len(work):
                issue_load(wi + PF)
            grp = groups[gi]
            b = grp[0][0]
            offs = []
            o = 0
            for (_, r, t0, n) in grp:
                offs.append(o)
                o += n
            if e == 0 and kind == "gu" and idx == 0:
                prologue(grp, offs, b)
            slot = ring[wi % 4]
            if kind == "gu":
                j = idx
                sv = slot.ap.rearrange("p (g c f) -> p g c f", g=2, c=8)
                for hc in range(4):
                    for (_, r, t0, n), o in zip(grp, offs):
                        pg = P.psA()
                        for c in range(8):
                            P.mm(pg, pg.ap[:, 0:n], sv[:, 0, c, hc * 128:(hc + 1) * 128], u.ap[:, c, o:o + n],
                                 [slot, u], c == 0, c == 7)
                        pu = P.psA()
                        for c in range(8):
                            P.mm(pu, pu.ap[:, 0:n], sv[:, 1, c, hc * 128:(hc + 1) * 128], u.ap[:, c, o:o + n],
                                 [slot, u], c == 0, c == 7)
                        s_ = sg[isg % 2]
                        P.act(s_.ap[:, 0:n], pg.ap[:, 0:n], AF.Silu, [pg], [s_])
                        if moe:
                            t_ = t2[isg % 2]
                            P.tt("dve", t_.ap[:, 0:n], s_.ap[:, 0:n], pu.ap[:, 0:n], ALU.mult, [s_, pu], [t_])
                            P.tt("dve", hid.ap[:, j * 4 + hc, o:o + n], t_.ap[:, 0:n], gbc.ap[:, e, o:o + n],
                                 ALU.mult, [t_, gbc], [hid])
                        else:
                            P.tt("dve", hid.ap[:, j * 4 + hc, o:o + n], s_.ap[:, 0:n], pu.ap[:, 0:n], ALU.mult,
                                 [s_, pu], [hid])
                        isg += 1
            else:
                ds = idx
                sv = slot.ap[:, 0:28 * 256].rearrange("p (c f) -> p c f", c=28)
                for o2 in range(2):
                    oc = ds * 2 + o2
                    for (_, r, t0, n), o in zip(grp, offs):
                        pd = P.psB()
                        for c in range(28):
                            P.mm(pd, pd.ap[:, 0:n], sv[:, c, o2 * 128:(o2 + 1) * 128], hid.ap[:, c, o:o + n],
                                 [slot, hid], c == 0, c == 27)
                        P.stt(hres.ap[:, oc, o:o + n], pd.ap[:, 0:n], k.modT.ap[:, i, 40 + oc, r:r + 1],
                              hres.ap[:, oc, o:o + n], ALU.mult, ALU.add, [pd, k.modT, hres], [hres])
                if e == nexp - 1 and ds == 3:
                    for (_, r, t0, n), o in zip(grp, offs):
                        P.dma("act", k.hT[b, :, t0:t0 + n].rearrange("(c p) t -> p c t", p=128),
                              hres.ap[:, :, o:o + n], [hres], hdeps(k.hTd, b, t0, n))
    P.barrier()


def mixer_pool(k, i, last):
    P = k.P
    wins = (2, 4, 8, 16)
    with ExitStack() as es:
        win = P.sb(es, "p_win", [128, 8, D], BF16)
        for kc in range(0, 8, 4):
            load_w_bf16(k, win, win.ap[:, kc:kc + 4, :],
                        k.w["pool_w_in"][0, kc * 128:(kc + 4) * 128, :].rearrange("(c p) f -> p c f", p=128))
        wg = P.sb(es, "p_wg", [128, 4, 2, 256], BF16)
        for g in range(4):
            load_w_bf16(k, wg, wg.ap[:, g], k.w["pool_w_grp"][0, g].rearrange("(c p) o -> p c o", p=128))
        psc = P.sb(es, "p_psc", [128, 8], F32)
        pr = P.sb(es, "p_pr", [8, 128], F32)
        P.dma("sp", pr.ap, k.w["pool_scale"][0].rearrange("(c p) -> c p", p=128), [], [pr])
        colvec(k, psc, psc.ap, pr, pr.ap, 8)
        gs = P.sb(es, "p_gs", [128, 8, 3], F32)
        P.tt("dve", gs.ap, k.modT.ap[:, i, 16:24, :], psc.ap.unsqueeze(2).to_broadcast([128, 8, 3]), ALU.mult,
             [k.modT, psc], [gs])
        rcL = P.sb(es, "p_rcL", [128, 4, SEQ], F32)
        rcC = P.sb(es, "p_rcC", [128, 4, CTX], F32)
        with ExitStack() as es2:
            ti = P.sb(es2, "p_ti", [128, SEQ], I32)
            tf = P.sb(es2, "p_tf", [128, SEQ], F32)
            hi = P.sb(es2, "p_hi", [128, SEQ], F32)
            P.op("pool", lambda g: g.iota(out=ti.ap, pattern=[[1, SEQ]], base=0, channel_multiplier=0), [], [ti])
            P.copy("dve", tf.ap, ti.ap, [ti], [tf])
            for (rc, n) in ((rcL, SEQ), (rcC, CTX)):
                for wi_, w in enumerate(wins):
                    P.ts("dve", hi.ap[:, 0:n], tf.ap[:, 0:n], float(w // 2), float(n), ALU.add, ALU.min, [tf], [hi])
                    P.ts("dve", rc.ap[:, wi_, :], tf.ap[:, 0:n], float(-(w // 2)), 0.0, ALU.add, ALU.max, [tf], [rc])
                    P.tt("dve", rc.ap[:, wi_, :], hi.ap[:, 0:n], rc.ap[:, wi_, :], ALU.subtract, [hi, rc], [rc])
                    P.op("dve", lambda g: g.reciprocal(out=rc.ap[:, wi_, :], in_=rc.ap[:, wi_, :]), [rc], [rc])
        P.barrier()
        us = P.sb(es, "p_us", [128, 8, SEQ], BF16)
        vps = [P.sb(es, f"p_vp{j}", [128, SEQ + 16], F32) for j in range(2)]
        sA = P.sb(es, "p_sA", [128, SEQ + 16], F32)
        sB = P.sb(es, "p_sB", [128, SEQ + 16], F32)
        tmp = P.sb(es, "p_tmp", [128, SEQ], F32)
        pl = P.sb(es, "p_pl", [128, 8, SEQ], BF16)
        hb = [P.sb(es, f"p_h{j}", [128, 8, 512], F32) for j in range(2)]
        ih = 0
        iv = 0
        for b in range(NB):
            segs = [(CTX, SEQ, b, rcL)] if last else [(0, CTX, 2, rcC), (CTX, SEQ, b, rcL)]
            for (tok0, n, r, rc) in segs:
                P.dma("sp", us.ap[:, :, 0:n], k.uT[b, :, tok0:tok0 + n].rearrange("(c p) t -> p c t", p=128),
                      hdeps(k.uTd, b, tok0, n), [us])
                for fc in range(8):
                    g = fc // 2
                    w = wins[g]
                    vp = vps[iv % 2]
                    iv += 1
                    P.memset("pool", vp, vp.ap[:, 0:8], 0.0)
                    P.memset("pool", vp, vp.ap[:, 8 + n:16 + n], 0.0)
                    for t0 in range(0, n, 512):
                        N = min(512, n - t0)
                        ps = P.psA()
                        for kc in range(8):
                            P.mm(ps, ps.ap[:, 0:N], win.ap[:, kc, fc * 128:(fc + 1) * 128], us.ap[:, kc, t0:t0 + N],
                                 [win, us], kc == 0, kc == 7)
                        P.copy("act", vp.ap[:, 8 + t0:8 + t0 + N], ps.ap[:, 0:N], [ps], [vp])
                    S, L, step, nb_ = vp, n + 16, 1, 0
                    while 2 * step <= w:
                        nxt = sA if nb_ % 2 == 0 else sB
                        nb_ += 1
                        L = L - step
                        P.tt("dve", nxt.ap[:, 0:L], S.ap[:, 0:L], S.ap[:, step:step + L], ALU.add, [S], [nxt])
                        S = nxt
                        step *= 2
                    o = 8 - w // 2
                    P.tt(k.pool_eng, tmp.ap[:, 0:n], S.ap[:, o:o + n], rc.ap[:, g, 0:n], ALU.mult, [S, rc], [tmp])
                    P.tt("dve", pl.ap[:, fc, 0:n], tmp.ap[:, 0:n], vp.ap[:, 8:8 + n], ALU.subtract, [tmp, vp], [pl])
                    if k.dbg_barrier:
                        P.barrier()
                for t0 in range(0, n, 512):
                    N = min(512, n - t0)
                    h = hb[ih % 2]
                    ih += 1
                    P.dma("sp", h.ap[:, :, 0:N], k.hT[b, :, tok0 + t0:tok0 + t0 + N].rearrange("(c p) t -> p c t", p=128),
                          hdeps(k.hTd, b, tok0 + t0, N), [h])
                    for fo in range(8):
                        g, oc = fo // 2, fo % 2
                        ps = P.psB()
                        for ic in range(2):
                            P.mm(ps, ps.ap[:, 0:N], wg.ap[:, g, ic, oc * 128:(oc + 1) * 128],
                                 pl.ap[:, 2 * g + ic, t0:t0 + N], [wg, pl], ic == 0, ic == 1)
                        P.stt(h.ap[:, fo, 0:N], ps.ap[:, 0:N], gs.ap[:, fo, r:r + 1], h.ap[:, fo, 0:N], ALU.mult, ALU.add,
                              [ps, gs, h], [h])
                    P.dma("act", k.hT[b, :, tok0 + t0:tok0 + t0 + N].rearrange("(c p) t -> p c t", p=128), h.ap[:, :, 0:N],
                          [h], hdeps(k.hTd, b, tok0 + t0, N))
    P.barrier()


def mixer_lru(k, i, last):
    P = k.P
    LW = 1280
    with ExitStack() as es:
        win = P.sb(es, "l_win", [128, 8, 2 * LW], BF16)
        for s5 in range(5):
            load_w_bf16(k, win, win.ap[:, :, s5 * 512:(s5 + 1) * 512],
                        k.w["lru_w_in"][0, :, s5 * 512:(s5 + 1) * 512].rearrange("(c p) f -> p c f", p=128))
        gw = P.sb(es, "l_gw", [128, 2, 2, 10, 128], BF16)
        for d in range(2):
            for j in range(2):
                load_w_bf16(k, gw, gw.ap[:, d, j], k.w["lru_gate_w"][0, d, j].rearrange("c p o -> p c o"))
        vecs = P.sb(es, "l_vecs", [128, 110], F32)
        vst = P.sb(es, "l_vst", [110, 128], F32)
        P.dma("sp", vst.ap[0:40, :], k.w["lru_conv_w"][0].rearrange("j (c p) -> (j c) p", p=128), [], [vst])
        P.dma("sp", vst.ap[40:50, :], k.w["lru_conv_b"][0].rearrange("(c p) -> c p", p=128), [], [vst])
        P.dma("sp", vst.ap[50:90, :], k.w["lru_gate_b"][0].rearrange("d j (c p) -> (d j c) p", p=128), [], [vst])
        P.dma("sp", vst.ap[90:110, :], k.w["lru_lambda"][0].rearrange("d (c p) -> (d c) p", p=128), [], [vst])
        colvec(k, vecs, vecs.ap, vst, vst.ap, 110)
        cw = vecs.ap[:, 0:40].rearrange("p (j c) -> p j c", j=4)
        cb = vecs.ap[:, 40:50]
        gbias = vecs.ap[:, 50:90].rearrange("p (d j c) -> p d j c", d=2, j=2)
        m8 = P.sb(es, "l_m8", [128, 20], F32)
        P.act(m8.ap, vecs.ap[:, 90:110], AF.Exp, [vecs], [m8], scale=-1.0)
        P.act(m8.ap, m8.ap, AF.Ln, [m8], [m8], bias=k.oneT.ap[:, 0:1], scale=1.0)
        P.ts("dve", m8.ap, m8.ap, -8.0, None, ALU.mult, None, [m8], [m8])
        u = P.sb(es, "l_u", [128, 8, TOK], BF16)
        XPW = 2312
        xp = P.sb(es, "l_xp", [128, XPW], F32)
        xr = P.sb(es, "l_xr", [128, TOK], F32)
        xrb = P.sb(es, "l_xrb", [128, TOK], BF16)
        gx = P.sb(es, "l_gx", [128, TOK], F32)
        A = P.sb(es, "l_A", [128, TOK], F32)
        Bx = P.sb(es, "l_Bx", [128, TOK], F32)
        tmp = P.sb(es, "l_tmp", [128, TOK], F32)
        hf = P.sb(es, "l_hf", [128, TOK], F32)
        hbk = P.sb(es, "l_hb", [128, TOK], F32)
        yb = P.sb(es, "l_yb", [128, TOK], BF16)
        P.memset("pool", xp, xp.ap, 0.0)
        segs = [(0, CTX, 2), (CTX, SEQ, 262)]
        tiles = [(0, CTX, 2)] + [(CTX + t, 512, 262 + t) for t in range(0, SEQ, 512)]
        for b in range(NB):
            P.dma("sp", u.ap, k.uT[b].rearrange("(c p) t -> p c t", p=128), hdeps(k.uTd, b, 0, TOK), [u])
            for ch in range(10):
                for (t0, N, xc) in tiles:
                    ps = P.psA()
                    for kc in range(8):
                        P.mm(ps, ps.ap[:, 0:N], win.ap[:, kc, LW + ch * 128:LW + (ch + 1) * 128], u.ap[:, kc, t0:t0 + N],
                             [win, u], kc == 0, kc == 7)
                    P.copy("act", xp.ap[:, xc:xc + N], ps.ap[:, 0:N], [ps], [xp])
                for (t0, n, s0) in segs:
                    P.ts("dve", xr.ap[:, t0:t0 + n], xp.ap[:, s0 - 2:s0 - 2 + n], cw[:, 0, ch:ch + 1], cb[:, ch:ch + 1],
                         ALU.mult, ALU.add, [xp, vecs], [xr])
                    for j in range(1, 4):
                        P.stt(xr.ap[:, t0:t0 + n], xp.ap[:, s0 - 2 + j:s0 - 2 + j + n], cw[:, j, ch:ch + 1],
                              xr.ap[:, t0:t0 + n], ALU.mult, ALU.add, [xp, vecs, xr], [xr])
                P.copy("act", xrb.ap, xr.ap, [xr], [xrb])
                for (t0, N, xc) in tiles:
                    ps = P.psA()
                    for kc in range(8):
                        P.mm(ps, ps.ap[:, 0:N], win.ap[:, kc, ch * 128:(ch + 1) * 128], u.ap[:, kc, t0:t0 + N],
                             [win, u], kc == 0, kc == 7)
                    P.copy("act", gx.ap[:, t0:t0 + N], ps.ap[:, 0:N], [ps], [gx])
                P.act(tmp.ap, gx.ap, AF.Square, [gx], [tmp])
                P.ts("dve", tmp.ap, tmp.ap, 0.044715, 1.0, ALU.mult, ALU.add, [tmp], [tmp])
                P.tt("dve", tmp.ap, tmp.ap, gx.ap, ALU.mult, [tmp, gx], [tmp])
                P.act(tmp.ap, tmp.ap, AF.Sigmoid, [tmp], [tmp], scale=1.5957691216057308)
                P.tt("dve", gx.ap, gx.ap, tmp.ap, ALU.mult, [gx, tmp], [gx])
                for d in range(2):
                    for j, dst in ((0, A), (1, Bx)):
                        for (t0, N, xc) in tiles:
                            ps = P.psB()
                            P.mm(ps, ps.ap[:, 0:N], gw.ap[:, d, j, ch, :], xrb.ap[:, t0:t0 + N], [gw, xrb], True, True)
                            P.act(dst.ap[:, t0:t0 + N], ps.ap[:, 0:N], AF.Sigmoid, [ps, vecs], [dst],
                                  bias=gbias[:, d, j, ch:ch + 1], scale=1.0)
                    P.act(A.ap, A.ap, AF.Exp, [A, m8], [A], scale=m8.ap[:, d * 10 + ch:d * 10 + ch + 1])
                    P.act(tmp.ap, A.ap, AF.Square, [A], [tmp])
                    P.act(tmp.ap, tmp.ap, AF.Sqrt, [tmp], [tmp], bias=k.oneT.ap[:, 0:1], scale=-1.0)
                    P.tt("dve", Bx.ap, Bx.ap, xr.ap, ALU.mult, [Bx, xr], [Bx])
                    P.tt("dve", Bx.ap, Bx.ap, tmp.ap, ALU.mult, [Bx, tmp], [Bx])
                    if d == 0:
                        P.op("dve", lambda g: g.tensor_tensor_scan(out=hf.ap, data0=A.ap, data1=Bx.ap, initial=0.0,
                                                                   op0=ALU.mult, op1=ALU.add), [A, Bx], [hf])
                    else:
                        P.op("dve", lambda g: g.tensor_tensor_scan(
                            out=hbk.ap[:, 0:CTX][:, ::-1], data0=A.ap[:, 0:CTX][:, ::-1], data1=Bx.ap[:, 0:CTX][:, ::-1],
                            initial=0.0, op0=ALU.mult, op1=ALU.add), [A, Bx], [hbk])
                        P.op("dve", lambda g: g.tensor_tensor_scan(
                            out=hbk.ap[:, CTX:TOK][:, ::-1], data0=A.ap[:, CTX:TOK][:, ::-1],
                            data1=Bx.ap[:, CTX:TOK][:, ::-1], initial=hbk.ap[:, 0:1], op0=ALU.mult, op1=ALU.add),
                            [A, Bx, hbk], [hbk])
                P.tt("dve", hf.ap, hf.ap, hbk.ap, ALU.add, [hf, hbk], [hf])
                P.tt("dve", yb.ap, hf.ap, gx.ap, ALU.mult, [hf, gx], [yb])
                P.dma("act", k.aT[b, ch * 128:(ch + 1) * 128, :], yb.ap, [yb], hdeps(k.aTd, b, 0, TOK))
    P.barrier()
    emit_outproj(k, i, k.w["lru_w_out"][0], 10, last)


def mixer_gqa(k, i, last):
    P = k.P
    HD = 64
    with ExitStack() as es:
        W = P.sb(es, "a_w", [128, 8, 1536], BF16)
        for s3 in range(3):
            load_w_bf16(k, W, W.ap[:, :, s3 * 512:(s3 + 1) * 512],
                        k.w["att_w_qkv"][0, :, s3 * 512:(s3 + 1) * 512].rearrange("(c p) f -> p c f", p=128))
        gst = P.sb(es, "a_gst", [2, HD], F32)
        P.dma("sp", gst.ap[0:1, :], k.w["att_q_norm"][0:1, :], [], [gst])
        P.dma("sp", gst.ap[1:2, :], k.w["att_k_norm"][0:1, :], [], [gst])
        gqk = P.sb(es, "a_gqk", [HD, 2], F32)
        colvec(k, gqk, gqk.ap, gst, gst.ap, 2, rows=HD)
        cos = P.sb(es, "a_cos", [HD, SEQ], F32)
        sin = P.sb(es, "a_sin", [HD, SEQ], F32)
        P.dma("sp", cos.ap, k.rope_cos, [], [cos])
        P.dma("sp", sin.ap, k.rope_sin, [], [sin])
        pm = P.sb(es, "a_pm", [HD, HD], F32)
        pm2 = P.sb(es, "a_pm2", [HD, HD], F32)
        P.op("pool", lambda g: g.affine_select(out=pm.ap, in_=k.onesF.ap[0:HD, 0:HD], pattern=[[-1, HD]],
                                               compare_op=ALU.is_equal, fill=0.0, base=32, channel_multiplier=1),
             [k.onesF], [pm])
        P.op("pool", lambda g: g.affine_select(out=pm2.ap, in_=k.onesF.ap[0:HD, 0:HD], pattern=[[-1, HD]],
                                               compare_op=ALU.is_equal, fill=0.0, base=-32, channel_multiplier=1),
             [k.onesF], [pm2])
        P.tt("dve", pm.ap, pm.ap, pm2.ap, ALU.subtract, [pm, pm2], [pm])
        u = P.sb(es, "a_u", [128, 8, TOK], BF16)
        raw = P.sb(es, "a_raw", [HD, TOK], F32)
        sq = P.sb(es, "a_sq", [HD, TOK], F32)
        rsd = P.sb(es, "a_rsd", [HD, TOK], F32)
        hat = P.sb(es, "a_hat", [HD, TOK], F32)
        KT = P.sb(es, "a_KT", [HD, TOK], BF16)
        QT = [P.sb(es, f"a_QT{j}", [HD, TOK], BF16) for j in range(2)]
        V = P.sb(es, "a_V", [128, 18, HD], BF16)
        t1 = P.sb(es, "a_t1", [HD, 512], F32)
        t2 = P.sb(es, "a_t2", [HD, 512], F32)
        pts = [P.sb(es, f"a_pt{j}", [128, 512], BF16) for j in range(3)]
        rden = P.sb(es, "a_rden", [HD, 512], F32)
        ots = [P.sb(es, f"a_ot{j}", [HD, 512], BF16) for j in range(2)]
        tiles = [(0, CTX)] + [(CTX + t, 512) for t in range(0, SEQ, 512)]
        ipt = 0
        iq = 0

        def project(col0, gain_col, dst):
            for (t0, N) in tiles:
                ps = P.psA()
                for kc in range(8):
                    P.mm(ps, ps.ap[0:HD, 0:N], W.ap[:, kc, col0:col0 + HD], u.ap[:, kc, t0:t0 + N], [W, u], kc == 0, kc == 7)
                P.copy("act", raw.ap[:, t0:t0 + N], ps.ap[0:HD, 0:N], [ps], [raw])
            P.act(sq.ap, raw.ap, AF.Square, [raw], [sq])
            for (t0, N) in tiles:
                ps = P.psA()
                P.mm(ps, ps.ap[0:HD, 0:N], k.onesF.ap[0:HD, 0:HD], sq.ap[:, t0:t0 + N], [k.onesF, sq], True, True)
                P.act(rsd.ap[:, t0:t0 + N], ps.ap[0:HD, 0:N], AF.Sqrt, [ps], [rsd], bias=k.epsT.ap[0:HD, 0:1], scale=1.0 / HD)
            P.op("dve", lambda g: g.reciprocal(out=rsd.ap, in_=rsd.ap), [rsd], [rsd])
            P.stt(hat.ap, raw.ap, gqk.ap[:, gain_col:gain_col + 1], rsd.ap, ALU.mult, ALU.mult, [raw, gqk, rsd], [hat])
            P.copy("act", dst.ap[:, 0:CTX], hat.ap[:, 0:CTX], [hat], [dst])
            for t in range(0, SEQ, 512):
                ps = P.psA()
                P.mm(ps, ps.ap[0:HD, 0:512], pm.ap, hat.ap[:, CTX + t:CTX + t + 512], [pm, hat], True, True)
                P.tt("dve", t1.ap, hat.ap[:, CTX + t:CTX + t + 512], cos.ap[:, t:t + 512], ALU.mult, [hat, cos], [t1])
                P.tt("dve", t2.ap, ps.ap[0:HD, 0:512], sin.ap[:, t:t + 512], ALU.mult, [ps, sin], [t2])
                P.tt("dve", dst.ap[:, CTX + t:CTX + t + 512], t1.ap, t2.ap, ALU.add, [t1, t2], [dst])

        for b in range(NB):
            P.dma("sp", u.ap, k.uT[b].rearrange("(c p) t -> p c t", p=128), hdeps(k.uTd, b, 0, TOK), [u])
            for g in range(4):
                project(1024 + g * HD, 1, KT)
                for b0 in range(0, 18, 8):
                    nb = min(8, 18 - b0)
                    ps = P.psA()
                    for j in range(nb):
                        for kc in range(8):
                            P.mm(ps, ps.ap[:, j * HD:(j + 1) * HD], u.ap[:, kc, (b0 + j) * 128:(b0 + j + 1) * 128],
                                 W.ap[:, kc, 1280 + g * HD:1280 + (g + 1) * HD], [W, u], kc == 0, kc == 7)
                    P.copy("act", V.ap[:, b0:b0 + nb, :], ps.ap[:, 0:nb * HD].rearrange("p (j d) -> p j d", d=HD), [ps], [V])
                for r in range(4):
                    h = g * 4 + r
                    Q = QT[h % 2]
                    project(h * HD, 0, Q)
                    qtiles = [(CTX + t, 512, 0, 18) for t in range(0, SEQ, 512)]
                    if not last:
                        qtiles.append((0, CTX, 0, 2))
                    for (q0, N, kb0, kb1) in qtiles:
                        po = P.ps[4 + 2 * (iq % 2)]
                        pd = P.ps[5 + 2 * (iq % 2)]
                        ot = ots[iq % 2]
                        iq += 1
                        for kb in range(kb0, kb1):
                            pS = P.psA()
                            P.mm(pS, pS.ap[:, 0:N], KT.ap[:, kb * 128:(kb + 1) * 128], Q.ap[:, q0:q0 + N], [KT, Q], True, True)
                            pt = pts[ipt % 3]
                            ipt += 1
                            P.act(pt.ap[:, 0:N], pS.ap[:, 0:N], AF.Exp, [pS], [pt], scale=0.125)
                            P.mm(po, po.ap[0:HD, 0:N], V.ap[:, kb, :], pt.ap[:, 0:N], [V, pt], kb == kb0, kb == kb1 - 1)
                            P.mm(pd, pd.ap[0:HD, 0:N], k.onesB.ap[:, 0:HD], pt.ap[:, 0:N], [k.onesB, pt], kb == kb0, kb == kb1 - 1)
                        P.op("dve", lambda g_: g_.reciprocal(out=rden.ap[:, 0:N], in_=pd.ap[0:HD, 0:N]), [pd], [rden])
                        P.tt("dve", ot.ap[:, 0:N], po.ap[0:HD, 0:N], rden.ap[:, 0:N], ALU.mult, [po, rden], [ot])
                        P.dma("act", k.aT[b, h * HD:(h + 1) * HD, q0:q0 + N], ot.ap[:, 0:N], [ot], hdeps(k.aTd, b, q0, N))
    P.barrier()
    emit_outproj(k, i, k.w["att_w_out"][0], 8, last)


def bcast_rows(k, dst, dst_ap, row, row_ap, n):
    P = k.P
    for c0 in range(0, n, 512):
        m = min(512, n - c0)
        ps = P.psC()
        P.mm(ps, ps.ap[:, 0:m], k.onesF.ap[0:1, :], row_ap[:, c0:c0 + m], [k.onesF, row], True, True)
        P.copy("dve", dst_ap[:, c0:c0 + m], ps.ap[:, 0:m], [ps], [dst])


def mixer_ssd(k, i, last):
    P = k.P
    Win = k.w["ssm_w_in"][0]
    XPW = 2312
    with ExitStack() as es:
        Wdt = P.sb(es, "s_wdt", [128, 8, 64], BF16)
        load_w_bf16(k, Wdt, Wdt.ap, Win[:, 5120:5184].rearrange("(c p) f -> p c f", p=128))
        cst = P.sb(es, "s_cst", [120, 128], F32)
        P.dma("sp", cst.ap[0:96, :], k.w["ssm_conv_w"][0].rearrange("j (c p) -> (j c) p", p=128), [], [cst])
        P.dma("sp", cst.ap[96:120, :], k.w["ssm_conv_b"][0].rearrange("(c p) -> c p", p=128), [], [cst])
        cvec = P.sb(es, "s_cvec", [128, 120], F32)
        colvec(k, cvec, cvec.ap, cst, cst.ap, 120)
        cw = cvec.ap[:, 0:96].rearrange("p (j c) -> p j c", j=4)
        cb = cvec.ap[:, 96:120]
        row = P.sb(es, "s_row", [1, 160 + 2048], F32)
        P.dma("sp", row.ap[:, 0:64], k.w["ssm_dt_bias"][0:1].rearrange("o d h -> o (d h)"), [], [row])
        P.dma("sp", row.ap[:, 64:128], k.w["ssm_a_log"][0:1].rearrange("o d h -> o (d h)"), [], [row])
        P.dma("sp", row.ap[:, 128:160], k.w["ssm_d"][0:1, :], [], [row])
        P.dma("sp", row.ap[:, 160:160 + 2048], k.w["ssm_norm_g"][0:1, :], [], [row])
        rep = P.sb(es, "s_rep", [128, 160], F32)
        bcast_rows(k, rep, rep.ap, row, row.ap[:, 0:160], 160)
        P.act(rep.ap[:, 64:128], rep.ap[:, 64:128], AF.Exp, [rep], [rep])
        P.ts("dve", rep.ap[:, 64:128], rep.ap[:, 64:128], -1.0, None, ALU.mult, None, [rep], [rep])
        ngR = P.sb(es, "s_ng", [128, 2048], F32)
        bcast_rows(k, ngR, ngR.ap, row, row.ap[:, 160:160 + 2048], 2048)
        ssq = P.sb(es, "s_ssq", [128, NB, 18], F32)
        dtbR = rep.ap[:, 0:64].rearrange("p (d h) -> p d h", d=2)
        negR = rep.ap[:, 64:128].rearrange("p (d h) -> p d h", d=2)
        tiles = [(0, CTX, 2)] + [(CTX + t, 512, 262 + t) for t in range(0, SEQ, 512)]
        segs = [(0, CTX, 2), (CTX, SEQ, 262)]
        with ExitStack() as esg:
            Wg = P.sb(esg, "s_wg", [128, 8, 1280], BF16)
            xs_tm = P.sb(esg, "s_xs", [128, 18, 512], F32)
            BT = P.sb(esg, "s_BT", [128, TOK], BF16)
            CT = P.sb(esg, "s_CT", [128, TOK], BF16)
            Btm = P.sb(esg, "s_Btm", [128, 18, 128], BF16)
            zs = P.sb(esg, "s_zs", [128, 18, 512], BF16)
            dt = P.sb(esg, "s_dt", [128, 18, 2, 8], F32)
            la = P.sb(esg, "s_la", [128, 18, 2, 8], F32)
            for g in range(4):
                load_w_bf16(k, Wg, Wg.ap[:, :, 0:512], Win[:, g * 512:(g + 1) * 512].rearrange("(c p) f -> p c f", p=128))
                load_w_bf16(k, Wg, Wg.ap[:, :, 512:1024],
                            Win[:, 2048 + g * 512:2048 + (g + 1) * 512].rearrange("(c p) f -> p c f", p=128))
                load_w_bf16(k, Wg, Wg.ap[:, :, 1024:1152],
                            Win[:, 4096 + g * 128:4096 + (g + 1) * 128].rearrange("(c p) f -> p c f", p=128))
                load_w_bf16(k, Wg, Wg.ap[:, :, 1152:1280],
                            Win[:, 4608 + g * 128:4608 + (g + 1) * 128].rearrange("(c p) f -> p c f", p=128))
                for b in range(NB):
                    with ExitStack() as ea:
                        u = P.sb(ea, "s_u", [128, 8, TOK], BF16)
                        xps = [P.sb(ea, f"s_xp{j}", [128, XPW], F32) for j in range(2)]
                        cv = P.sb(ea, "s_cv", [128, TOK], F32)
                        sTs = [P.sb(ea, f"s_sT{j}", [128, TOK], F32) for j in range(2)]
                        for xp in xps:
                            P.memset("pool", xp, xp.ap, 0.0)
                        P.dma("sp", u.ap, k.uT[b].rearrange("(c p) t -> p c t", p=128), hdeps(k.uTd, b, 0, TOK), [u])
                        jobs = [(512 + c * 128, g * 4 + c, "x", c) for c in range(4)] + [(1024, 16 + g, "B", 0), (1152, 20 + g, "C", 0)]
                        for ji, (wcol, cch, kind, c) in enumerate(jobs):
                            xp = xps[ji % 2]
                            sT = sTs[ji % 2]
                            for (t0, N, xc) in tiles:
                                ps = P.psA()
                                for kc in range(8):
                                    P.mm(ps, ps.ap[:, 0:N], Wg.ap[:, kc, wcol:wcol + 128], u.ap[:, kc, t0:t0 + N], [Wg, u],
                                         kc == 0, kc == 7)
                                P.copy("act", xp.ap[:, xc:xc + N], ps.ap[:, 0:N], [ps], [xp])
                            for (t0, n, s0) in segs:
                                P.ts("dve", cv.ap[:, t0:t0 + n], xp.ap[:, s0 - 2:s0 - 2 + n], cw[:, 0, cch:cch + 1],
                                     cb[:, cch:cch + 1], ALU.mult, ALU.add, [xp, cvec], [cv])
                                for j in range(1, 4):
                                    P.stt(cv.ap[:, t0:t0 + n], xp.ap[:, s0 - 2 + j:s0 - 2 + j + n], cw[:, j, cch:cch + 1],
                                          cv.ap[:, t0:t0 + n], ALU.mult, ALU.add, [xp, cvec, cv], [cv])
                            if kind == "C":
                                P.act(CT.ap, cv.ap, AF.Silu, [cv], [CT])
                                continue
                            P.act(sT.ap, cv.ap, AF.Silu, [cv], [sT])
                            if kind == "B":
                                P.copy("dve", BT.ap, sT.ap, [sT], [BT])
                            for b0 in range(0, 18, 4):
                                nb = min(4, 18 - b0)
                                ps = P.psB()
                                for j in range(nb):
                                    P.tr(ps, ps.ap[:, j * 128:(j + 1) * 128], sT.ap[:, (b0 + j) * 128:(b0 + j + 1) * 128],
                                         k.identF, [sT], signal=(j == nb - 1))
                                src = ps.ap[:, 0:nb * 128].rearrange("p (j f) -> p j f", f=128)
                                if kind == "x":
                                    P.copy("act" if (b0 // 4) % 2 == 0 else "dve", xs_tm.ap[:, b0:b0 + nb, c * 128:(c + 1) * 128],
                                           src, [ps], [xs_tm])
                                else:
                                    P.copy("dve", Btm.ap[:, b0:b0 + nb, :], src, [ps], [Btm])
                        for blk in range(18):
                            ps = P.psA()
                            for kc in range(8):
                                P.mm(ps, ps.ap, u.ap[:, kc, blk * 128:(blk + 1) * 128], Wg.ap[:, kc, 0:512], [Wg, u],
                                     kc == 0, kc == 7)
                            P.act(zs.ap[:, blk, :], ps.ap, AF.Silu, [ps], [zs])
                        psd = P.psC()
                        for blk in range(18):
                            for d in range(2):
                                for kc in range(8):
                                    P.mm(psd, psd.ap[:, blk * 16 + d * 8:blk * 16 + d * 8 + 8], u.ap[:, kc, blk * 128:(blk + 1) * 128],
                                         Wdt.ap[:, kc, d * 32 + g * 8:d * 32 + g * 8 + 8], [Wdt, u], kc == 0, kc == 7)
                        P.tt("dve", dt.ap, psd.ap[:, 0:288].rearrange("p (b d r) -> p b d r", d=2, r=8),
                             dtbR[:, :, g * 8:(g + 1) * 8].unsqueeze(1).to_broadcast([128, 18, 2, 8]), ALU.add, [psd, rep], [dt])
                        P.ts("dve", la.ap, dt.ap, -1.0, None, ALU.mult, None, [dt], [la])
                        P.tt("dve", la.ap, la.ap, dt.ap, ALU.min, [la, dt], [la])
                        P.act(la.ap, la.ap, AF.Exp, [la], [la])
                        P.act(la.ap, la.ap, AF.Ln, [la], [la], bias=k.oneT.ap[:, 0:1], scale=1.0)
                        P.stt(dt.ap, dt.ap, 0.0, la.ap, ALU.max, ALU.add, [dt, la], [dt])
                        P.tt("dve", la.ap, dt.ap, negR[:, :, g * 8:(g + 1) * 8].unsqueeze(1).to_broadcast([128, 18, 2, 8]),
                             ALU.mult, [dt, rep], [la])
                        if k.debug and g == 1 and b == 1:
                            P.dma("act", k.dbg[:, 0:288], dt.ap.rearrange("p a d r -> p (a d r)"), [dt], [k.dbgd])
                            P.dma("act", k.dbg[:, 288:576], la.ap.rearrange("p a d r -> p (a d r)"), [la], [k.dbgd])
                    P.barrier()
                    with ExitStack() as eb:
                        yacc = P.sb(eb, "s_yacc", [128, 18, 512], F32)
                        H = P.sb(eb, "s_H", [128, 8, 64], F32)
                        Hb = P.sb(eb, "s_Hb", [128, 512], BF16)
                        xdts = [P.sb(eb, f"s_xdt{j}", [128, 8, 64], BF16) for j in range(2)]
                        xws = [P.sb(eb, f"s_xw{j}", [128, 8, 64], BF16) for j in range(2)]
                        cbms = [P.sb(eb, f"s_cbm{j}", [128, 128], F32) for j in range(2)]
                        E3s = [P.sb(eb, f"s_E3{j}", [128, 24], F32) for j in range(2)]
                        rDs = [P.sb(eb, f"s_rD{j}", [128, 4, 128], F32) for j in range(2)]
                        EDs = [P.sb(eb, f"s_ED{j}", [128, 4, 128], F32) for j in range(2)]
                        MTs = [P.sb(eb, f"s_MT{j}", [128, 8, 128], BF16) for j in range(2)]
                        tmpo = [P.sb(eb, f"s_to{j}", [128, 8, 64], F32) for j in range(2)]
                        it = 0
                        ih = 0
                        for d in range(2):
                            strict, tri = (k.m_gt, k.m_le) if d == 0 else (k.m_lt, k.m_ge)
                            order = [0, 1] + list(range(2, 18)) if d == 0 else [1, 0] + list(range(17, 1, -1))
                            P.memset("dve", H, H.ap, 0.0)
                            P.memset("dve", Hb, Hb.ap, 0.0)
                            for c in order:
                                xdt, xw, cbm, E3, MT, to = xdts[it % 2], xws[it % 2], cbms[it % 2], E3s[it % 2], MTs[it % 2], tmpo[it % 2]
                                it += 1
                                lac = la.ap[:, c, d, :]
                                xsv = xs_tm.ap[:, c, :].rearrange("p (r q) -> p r q", q=64)
                                P.tt("dve", xdt.ap, xsv, dt.ap[:, c, d, :].unsqueeze(2).to_broadcast([128, 8, 64]), ALU.mult,
                                     [xs_tm, dt], [xdt])
                                pcb = P.psA()
                                P.mm(pcb, pcb.ap[:, 0:128], BT.ap[:, c * 128:(c + 1) * 128], CT.ap[:, c * 128:(c + 1) * 128],
                                     [BT, CT], True, True)
                                P.tt("dve", cbm.ap, pcb.ap[:, 0:128], tri.ap, ALU.mult, [pcb, tri], [cbm])
                                p3 = P.psC()
                                P.mm(p3, p3.ap[:, 0:8], tri.ap, lac, [tri, la], True, True)
                                P.mm(p3, p3.ap[:, 8:16], strict.ap, lac, [strict, la], True, True)
                                P.mm(p3, p3.ap[:, 16:24], k.onesF.ap, lac, [k.onesF, la], True, True)
                                P.act(E3.ap, p3.ap[:, 0:24], AF.Exp, [p3], [E3])
                                for half in range(2):
                                    rD, ED = rDs[ih % 2], EDs[ih % 2]
                                    ih += 1
                                    P.tt("dve", rD.ap, tri.ap.unsqueeze(1).to_broadcast([128, 4, 128]),
                                         lac[:, half * 4:half * 4 + 4].unsqueeze(2).to_broadcast([128, 4, 128]), ALU.mult,
                                         [tri, la], [rD])
                                    pD = P.psA()
                                    P.mm(pD, pD.ap, strict.ap, rD.ap.rearrange("p r l -> p (r l)"), [strict, rD], True, True)
                                    P.act(ED.ap.rearrange("p r l -> p (r l)"), pD.ap, AF.Exp, [pD], [ED])
                                    P.tt("dve", MT.ap[:, half * 4:half * 4 + 4, :], ED.ap,
                                         cbm.ap.unsqueeze(1).to_broadcast([128, 4, 128]), ALU.mult, [ED, cbm], [MT])
                                if k.debug and g == 0 and b == 0 and c in (5, 16):
                                    P.dma("act", k.dbg2[d * 2 + (0 if c == 5 else 1)], MT.ap.rearrange("p r l -> p (r l)"), [MT], [k.dbgd])
                                pY = P.psB()
                                for r in range(8):
                                    P.mm(pY, pY.ap[:, r * 64:(r + 1) * 64], MT.ap[:, r, :], xdt.ap[:, r, :], [MT, xdt], True, True)
                                pO = P.psB()
                                P.mm(pO, pO.ap, CT.ap[:, c * 128:(c + 1) * 128], Hb.ap, [CT, Hb], True, True)
                                P.tt("dve", to.ap, pO.ap.rearrange("p (r q) -> p r q", q=64),
                                     E3.ap[:, 0:8].unsqueeze(2).to_broadcast([128, 8, 64]), ALU.mult, [pO, E3], [to])
                                yv = yacc.ap[:, c, :].rearrange("p (r q) -> p r q", q=64)
                                pYv = pY.ap.rearrange("p (r q) -> p r q", q=64)
                                if d == 0:
                                    P.tt("dve", yv, pYv, to.ap, ALU.add, [pY, to], [yacc])
                                else:
                                    P.tt("dve", to.ap, pYv, to.ap, ALU.add, [pY, to], [to])
                                    P.tt("dve", yv, yv, to.ap, ALU.add, [yacc, to], [yacc])
                                P.tt("dve", xw.ap, xdt.ap, E3.ap[:, 8:16].unsqueeze(2).to_broadcast([128, 8, 64]), ALU.mult,
                                     [xdt, E3], [xw])
                                pS = P.psA()
                                P.mm(pS, pS.ap, Btm.ap[:, c, :], xw.ap.rearrange("p r q -> p (r q)"), [Btm, xw], True, True)
                                P.tt("dve", H.ap, H.ap, E3.ap[:, 16:24].unsqueeze(2).to_broadcast([128, 8, 64]), ALU.mult,
                                     [H, E3], [H])
                                P.tt("dve", H.ap, H.ap, pS.ap.rearrange("p (r q) -> p r q", q=64), ALU.add, [H, pS], [H])
                                P.copy("act", Hb.ap, H.ap.rearrange("p r q -> p (r q)"), [H], [Hb])
                                if k.dbg_barrier:
                                    P.barrier()
                            if k.debug and g == 1 and b == 1:
                                P.dma("act", k.dbg3[d], yacc.ap.rearrange("p c f -> p (c f)"), [yacc], [k.dbgd])
                            P.barrier()
                        ygs = [P.sb(eb, f"s_yg{j}", [128, 8, 64], F32) for j in range(2)]
                        ygb = [P.sb(eb, f"s_ygb{j}", [128, 512], BF16) for j in range(2)]
                        junk = P.sb(eb, "s_junk", [128, 512], F32)
                        col = P.sb(eb, "s_col", [128, 1], F32)
                        for c in range(18):
                            yg, yb_ = ygs[c % 2], ygb[c % 2]
                            xsv = xs_tm.ap[:, c, :].rearrange("p (r q) -> p r q", q=64)
                            P.tt("dve", yg.ap, xsv, rep.ap[:, 128 + g * 8:128 + (g + 1) * 8].unsqueeze(2).to_broadcast([128, 8, 64]),
                                 ALU.mult, [xs_tm, rep], [yg])
                            P.tt("dve", yg.ap, yg.ap, yacc.ap[:, c, :].rearrange("p (r q) -> p r q", q=64), ALU.add, [yg, yacc], [yg])
                            ygf = yg.ap.rearrange("p r q -> p (r q)")
                            P.tt("dve", ygf, ygf, zs.ap[:, c, :], ALU.mult, [yg, zs], [yg])
                            P.copy("act", yb_.ap, ygf, [yg], [yb_])
                            P.tt("dve", junk.ap, ygf, ygf, ALU.mult, [yg], [junk])
                            if g == 0:
                                P.op("dve", lambda e_: e_.reduce_sum(out=ssq.ap[:, b, c:c + 1], in_=junk.ap, axis=AX.X), [junk], [ssq])
                            else:
                                P.op("dve", lambda e_: e_.reduce_sum(out=col.ap, in_=junk.ap, axis=AX.X), [junk], [col])
                                P.tt("dve", ssq.ap[:, b, c:c + 1], ssq.ap[:, b, c:c + 1], col.ap, ALU.add, [ssq, col], [ssq])
                            P.dma("act", k.yS[b, c * 128:(c + 1) * 128, g * 512:(g + 1) * 512], yb_.ap, [yb_], [k.ySd[b][c]])
                    P.barrier()
        with ExitStack() as ef:
            wo = P.sb(ef, "s_wo", [128, 16, D], BF16)
            for kc in range(0, 16, 4):
                load_w_bf16(k, wo, wo.ap[:, kc:kc + 4, :],
                            k.w["ssm_w_out"][0, kc * 128:(kc + 4) * 128, :].rearrange("(c p) f -> p c f", p=128))
            rst = P.sb(ef, "s_rst", [128, NB, 18], F32)
            P.act(rst.ap, ssq.ap, AF.Sqrt, [ssq], [rst], bias=k.epsT.ap[:, 0:1], scale=1.0 / 2048)
            P.op("dve", lambda e_: e_.reciprocal(out=rst.ap, in_=rst.ap), [rst], [rst])
            yin = [P.sb(ef, f"s_yin{j}", [128, 2048], BF16) for j in range(2)]
            yn = [P.sb(ef, f"s_yn{j}", [128, 2048], BF16) for j in range(2)]
            yT = [P.sb(ef, f"s_yT{j}", [128, 16, 512], BF16) for j in range(2)]
            hb = [P.sb(ef, f"s_h{j}", [128, 8, 512], F32) for j in range(2)]
            ib = 0
            for it, (b, r, t0, n) in enumerate(token_tiles(with_ctx=not last)):
                yt, h = yT[it % 2], hb[it % 2]
                P.dma("sp", h.ap[:, :, 0:n], k.hT[b, :, t0:t0 + n].rearrange("(c p) t -> p c t", p=128), hdeps(k.hTd, b, t0, n), [h])
                for j in range(n // 128):
                    blk = t0 // 128 + j
                    yi, yo = yin[ib % 2], yn[ib % 2]
                    ib += 1
                    P.dma("sp", yi.ap, k.yS[b, blk * 128:(blk + 1) * 128, :], [k.ySd[b][blk]], [yi])
                    P.stt(yo.ap, yi.ap, rst.ap[:, b, blk:blk + 1], ngR.ap, ALU.mult, ALU.mult, [yi, rst, ngR], [yo])
                    for k8 in range(2):
                        ps = P.psB()
                        pv = ps.ap.bitcast(BF16)
                        for q in range(8):
                            kc = k8 * 8 + q
                            P.tr(ps, pv[:, q * 128:(q + 1) * 128], yo.ap[:, kc * 128:(kc + 1) * 128], k.identB, [yo], signal=(q == 7))
                        P.copy("act" if k8 == 0 else "dve", yt.ap[:, k8 * 8:(k8 + 1) * 8, j * 128:(j + 1) * 128],
                               pv.rearrange("p (q t) -> p q t", t=128), [ps], [yt])
                for oc in range(8):
                    ps = P.psA()
                    for kc in range(16):
                        P.mm(ps, ps.ap[:, 0:n], wo.ap[:, kc, oc * 128:(oc + 1) * 128], yt.ap[:, kc, 0:n], [wo, yt], kc == 0, kc == 15)
                    P.stt(h.ap[:, oc, 0:n], ps.ap[:, 0:n], k.modT.ap[:, i, 16 + oc, r:r + 1], h.ap[:, oc, 0:n], ALU.mult, ALU.add,
                          [ps, k.modT, h], [h])
                P.dma("act", k.hT[b, :, t0:t0 + n].rearrange("(c p) t -> p c t", p=128), h.ap[:, :, 0:n], [h], hdeps(k.hTd, b, t0, n))
    P.barrier()


def emit_all(k, es):
    P = k.P
    emit_consts(k, es)
    k.epsT = P.sb(es, "epsT", [128, 1], F32)
    P.memset("dve", k.epsT, k.epsT.ap, EPS)
    k.oneT = P.sb(es, "oneT", [128, 1], F32)
    P.memset("dve", k.oneT, k.oneT.ap, 1.0)
    emit_mod(k, es)
    emit_load(k)
    st = k.stages
    for i in range(4):
        last = i == 3
        if st is None or ("mix%d" % i) in st:
            emit_modulate(k, i, last)
            MIXERS[i](k, i, last)
        if st is None or ("ffn%d" % i) in st:
            emit_ffn(k, i, last, moe=(i % 2 == 1))
    emit_store(k)


def mixer_todo(k, i, last):
    pass


MIXERS = [mixer_ssd, mixer_gqa, mixer_pool, mixer_lru]


def rope_tables():
    rows = SEQ // 64
    row = np.repeat(np.arange(rows), 64).astype(np.float32)
    col = np.tile(np.arange(64), rows).astype(np.float32)
    nf = 16
    inv = (10000.0 ** (-np.arange(nf, dtype=np.float32) / nf)).astype(np.float32)
    ang = np.concatenate([row[:, None] * inv, col[:, None] * inv], axis=-1).astype(np.float32)
    cos = np.cos(ang).astype(np.float32).T
    sin = np.sin(ang).astype(np.float32).T
    return (np.ascontiguousarray(np.concatenate([cos, cos], 0)), np.ascontiguousarray(np.concatenate([sin, sin], 0)))


def make_in_maps(inputs, n_cores=8, dummy=()):
    cos, sin = rope_tables()
    maps = []
    for ci in range(n_cores):
        m = {"x": np.ascontiguousarray(inputs["x"][ci * NB:(ci + 1) * NB]),
             "c": np.ascontiguousarray(inputs["c"][ci * NB:(ci + 1) * NB]),
             "ctx": np.ascontiguousarray(inputs["ctx"][ci * NB:(ci + 1) * NB]),
             "rope_cos": cos, "rope_sin": sin}
        for name, _ in WEIGHT_SPECS:
            m[name] = np.zeros([128], np.float32) if name in dummy else np.ascontiguousarray(inputs[name], dtype=np.float32)
        maps.append(m)
    return maps


def kernel(**inputs):
    nc, _ = build()
    maps = make_in_maps(inputs, 8)
    res = run_bass_kernel_spmd(nc, maps, core_ids=list(range(8)))
    return np.concatenate([np.asarray(r["out"]) for r in res.results], axis=0).astype(np.float32)
```

```python
import numpy as np
from contextlib import ExitStack
import concourse.bass as bass
import concourse.mybir as mybir
from concourse.bass_utils import run_bass_kernel_spmd

F32 = mybir.dt.float32
BF16 = mybir.dt.bfloat16
I32 = mybir.dt.int32
AF = mybir.ActivationFunctionType
ALU = mybir.AluOpType
AX = mybir.AxisListType

D = 1024
NB = 2
CTX = 256
SEQ = 2048
TOK = CTX + SEQ
EPS = 1e-6
FFN = 3584
NE = 8
NDS = 24


class Dep:
    __slots__ = ("w", "r")

    def __init__(self):
        self.w = None
        self.r = {}


class T:
    def __init__(self, ap, dep=None):
        self.ap = ap
        self.dep = dep if dep is not None else Dep()


def _deps_of(lst):
    out = []
    for x in lst:
        if isinstance(x, T):
            out.append(x.dep)
        elif isinstance(x, Dep):
            out.append(x)
        else:
            out.extend(_deps_of(x))
    return out


class Prog:
    def __init__(self, nc, es):
        self.nc = nc
        self.eng = {"pe": nc.tensor, "act": nc.scalar, "dve": nc.vector, "pool": nc.gpsimd, "sp": nc.sync}
        self.sem = {}
        self.cnt = {}
        self.waited = {k: {} for k in self.eng}
        for k in self.eng:
            self.sem[k] = es.enter_context(nc.semaphore("s_" + k))
            self.cnt[k] = 0
        self.dq = {}
        self.dqi = {}
        for q in ("sp", "act", "pool"):
            self.dq[q] = []
            self.dqi[q] = 0
            for i in range(NDS):
                key = ("d", q, i)
                self.sem[key] = es.enter_context(nc.semaphore(f"d_{q}{i}"))
                self.cnt[key] = 0
                self.dq[q].append(key)
        self.ps = [T(es.enter_context(nc.psum_tensor(f"ps{i}", [128, 512], F32))[:]) for i in range(8)]
        self.psi = {"A": 0, "B": 0, "C": 0}
        self.n_ins = 0

    def psA(self):
        self.psi["A"] += 1
        return self.ps[self.psi["A"] % 3]

    def psB(self):
        self.psi["B"] += 1
        return self.ps[4 + self.psi["B"] % 2]

    def psC(self):
        self.psi["C"] += 1
        return self.ps[6 + self.psi["C"] % 2]

    def sb(self, es, name, shape, dtype):
        self.uid = getattr(self, "uid", 0) + 1
        return T(es.enter_context(self.nc.sbuf_tensor(f"{name}_{self.uid}", list(shape), dtype))[:])

    def _wait(self, e, key, val):
        w = self.waited[e]
        if w.get(key, 0) >= val:
            return
        w[key] = val
        self.eng[e].wait_ge(self.sem[key], val)
        self.n_ins += 1

    def _deps(self, e, reads, writes, is_dma):
        deps = {}

        def add(k, v):
            if deps.get(k, 0) < v:
                deps[k] = v

        for d in reads:
            if d.w is not None:
                if not (e == "pe" and d.w[0] == "pe" and not is_dma):
                    add(*d.w)
        for d in writes:
            if d.w is not None and (is_dma or d.w[0] != e):
                add(*d.w)
            for k, v in d.r.items():
                if is_dma or k != e:
                    add(k, v)
        return deps

    def op(self, e, fn, reads=(), writes=(), signal=True):
        reads = _deps_of(reads)
        writes = _deps_of(writes)
        for k, v in self._deps(e, reads, writes, False).items():
            self._wait(e, k, v)
        ins = fn(self.eng[e])
        self.n_ins += 1
        if signal:
            self.cnt[e] += 1
            ins.then_inc(self.sem[e], 1)
            val = self.cnt[e]
        else:
            val = self.cnt[e] + 1
        for d in reads:
            if d.r.get(e, 0) < val:
                d.r[e] = val
        for d in writes:
            d.w = (e, val)
            d.r = {}
        return ins

    def dma(self, q, out, in_, reads=(), writes=(), **kw):
        reads = _deps_of(reads)
        writes = _deps_of(writes)
        key = self.dq[q][self.dqi[q] % NDS]
        self.dqi[q] += 1
        deps = self._deps(q, reads, writes, True)
        if self.cnt[key] > 0 and deps.get(key, 0) < self.cnt[key]:
            deps[key] = self.cnt[key]
        for k, v in deps.items():
            self._wait(q, k, v)
        ins = self.eng[q].dma_start(out=out, in_=in_, **kw)
        self.n_ins += 1
        self.cnt[key] += 16
        ins.then_inc(self.sem[key], 16)
        val = self.cnt[key]
        for d in reads:
            d.r[key] = val
        for d in writes:
            d.w = (key, val)
            d.r = {}

    def barrier(self):
        snap = dict(self.cnt)
        for e in self.eng:
            for k_, v in snap.items():
                if k_ != e and v > 0:
                    self._wait(e, k_, v)

    def finish(self):
        self.barrier()

    def mm(self, ps, out, lhsT, rhs, reads, start, stop):
        if lhsT.dtype == F32 and stop:
            self.op("pe", lambda e: e.matmul(out=out, lhsT=lhsT, rhs=rhs, start=start, stop=stop),
                    reads=reads, writes=[ps], signal=False)
            fb = self.ps[3]
            self.op("pe", lambda e: e.matmul(out=fb.ap[0:1, 0:2], lhsT=self.fence_src[0:1, 0:1],
                                             rhs=self.fence_src[0:1, 0:2], start=True, stop=True),
                    reads=[], writes=[ps], signal=True)
            return
        self.op("pe", lambda e: e.matmul(out=out, lhsT=lhsT, rhs=rhs, start=start, stop=stop),
                reads=reads, writes=[ps], signal=stop)

    def tr(self, ps, out, in_, ident, reads, signal=True, np_=128):
        self.op("pe", lambda e: e.transpose(out=out, in_=in_, identity=ident.ap[0:np_, 0:np_]),
                reads=list(reads) + [ident], writes=[ps], signal=signal)

    def act(self, out, in_, func, reads, writes, bias=None, scale=None, eng="act"):
        kw = {}
        if bias is not None:
            kw["bias"] = bias
        if scale is not None:
            kw["scale"] = scale
        self.op("act", lambda e: e.activation(out=out, in_=in_, func=func, **kw), reads=reads, writes=writes)

    def tt(self, e, out, in0, in1, op, reads, writes):
        self.op(e, lambda g: g.tensor_tensor(out=out, in0=in0, in1=in1, op=op), reads=reads, writes=writes)

    def ts(self, e, out, in0, s1, s2, op0, op1, reads, writes):
        if op1 is None:
            self.op(e, lambda g: g.tensor_scalar(out=out, in0=in0, scalar1=s1, scalar2=None, op0=op0),
                    reads=reads, writes=writes)
        else:
            self.op(e, lambda g: g.tensor_scalar(out=out, in0=in0, scalar1=s1, scalar2=s2, op0=op0, op1=op1),
                    reads=reads, writes=writes)

    def stt(self, out, in0, scalar, in1, op0, op1, reads, writes):
        self.op("dve", lambda g: g.scalar_tensor_tensor(out=out, in0=in0, scalar=scalar, in1=in1, op0=op0, op1=op1),
                reads=reads, writes=writes)

    def copy(self, e, out, in_, reads, writes):
        if e == "act":
            self.op("act", lambda g: g.copy(out=out, in_=in_), reads=reads, writes=writes)
        else:
            self.op(e, lambda g: g.tensor_copy(out=out, in_=in_), reads=reads, writes=writes)

    def memset(self, e, t, ap, val):
        self.op(e, lambda g: g.memset(ap, val), reads=[], writes=[t])


WEIGHT_SPECS = [
    ("c_ctx", [1024]), ("w_mod", [4, 1024, 6144]), ("b_mod", [4, 6144]), ("norm_g", [4, 2, 1024]),
    ("ssm_w_in", [1, 1024, 5184]), ("ssm_conv_w", [1, 4, 3072]), ("ssm_conv_b", [1, 3072]),
    ("ssm_dt_bias", [1, 2, 32]), ("ssm_a_log", [1, 2, 32]), ("ssm_d", [1, 32]), ("ssm_norm_g", [1, 2048]),
    ("ssm_w_out", [1, 2048, 1024]), ("att_w_qkv", [1, 1024, 1536]), ("att_q_norm", [1, 64]),
    ("att_k_norm", [1, 64]), ("att_w_out", [1, 1024, 1024]), ("pool_w_in", [1, 1024, 1024]),
    ("pool_w_grp", [1, 4, 256, 256]), ("pool_scale", [1, 1024]), ("lru_w_in", [1, 1024, 2560]),
    ("lru_conv_w", [1, 4, 1280]), ("lru_conv_b", [1, 1280]), ("lru_gate_w", [1, 2, 2, 10, 128, 128]),
    ("lru_gate_b", [1, 2, 2, 1280]), ("lru_lambda", [1, 2, 1280]), ("lru_w_out", [1, 1280, 1024]),
    ("ffn_w_gate", [2, 1024, 3584]), ("ffn_w_up", [2, 1024, 3584]), ("ffn_w_down", [2, 3584, 1024]),
    ("moe_w_router", [2, 1024, 8]), ("moe_w_gate", [2, 8, 1024, 3584]), ("moe_w_up", [2, 8, 1024, 3584]),
    ("moe_w_down", [2, 8, 3584, 1024]),
]


def token_tiles(b_list=(0, 1), with_ctx=True, width=512):
    out = []
    for b in b_list:
        if with_ctx:
            out.append((b, 2, 0, CTX))
        for t0 in range(0, SEQ, width):
            out.append((b, b, CTX + t0, min(width, SEQ - t0)))
    return out


class K:
    pass


def hdeps(arr, b, t0, n):
    return [arr[b][j] for j in range(t0 // 128, (t0 + n + 127) // 128)]


def build(stages=None, debug=False):
    nc = bass.Bass("TRN2", target_bir_lowering=False)
    k = K()
    k.nc = nc
    k.stages = stages
    import os
    k.dbg_barrier = bool(os.environ.get("DBG_BARRIER"))
    k.pool_eng = os.environ.get("POOL_ENG", "dve")
    dt = nc.dram_tensor
    k.x = dt("x", [NB, SEQ, D], F32, kind="ExternalInput").ap()
    k.c = dt("c", [NB, D], F32, kind="ExternalInput").ap()
    k.ctx = dt("ctx", [NB, CTX, D], F32, kind="ExternalInput").ap()
    k.w = {}
    k.dummy = set()
    pref = {"mix0": "ssm_", "mix1": "att_", "mix2": "pool_", "mix3": "lru_", "ffn0": "ffn_", "ffn2": "ffn_",
            "ffn1": "moe_", "ffn3": "moe_"}
    for name, shape in WEIGHT_SPECS:
        if stages is not None and name.split("_")[0] in ("ssm", "att", "pool", "lru", "ffn", "moe") and \
                not any(name.startswith(pref[s_]) for s_ in stages):
            k.dummy.add(name)
            shape = [128]
        k.w[name] = dt(name, shape, F32, kind="ExternalInput").ap()
    k.rope_cos = dt("rope_cos", [64, SEQ], F32, kind="ExternalInput").ap()
    k.rope_sin = dt("rope_sin", [64, SEQ], F32, kind="ExternalInput").ap()
    k.out = dt("out", [NB, SEQ, D], F32, kind="ExternalOutput").ap()
    k.hT = dt("hT", [NB, D, TOK], F32, kind="ExternalOutput" if debug else "Internal").ap()
    k.uT = dt("uT", [NB, D, TOK], BF16, kind="Internal").ap()
    k.aT = dt("aT", [NB, 2048, TOK], BF16, kind="Internal").ap()
    k.yS = dt("yS", [NB, TOK, 2048], BF16, kind="ExternalOutput" if debug else "Internal").ap()
    if debug:
        k.dbg = dt("dbg", [128, 576], F32, kind="ExternalOutput").ap()
        k.dbg2 = dt("dbg2", [4, 128, 1024], BF16, kind="ExternalOutput").ap()
        k.dbg3 = dt("dbg3", [2, 128, 9216], F32, kind="ExternalOutput").ap()
        k.dbgd = Dep()
    k.debug = debug
    k.hTd = [[Dep() for _ in range(18)] for _ in range(NB)]
    k.uTd = [[Dep() for _ in range(18)] for _ in range(NB)]
    k.aTd = [[Dep() for _ in range(18)] for _ in range(NB)]
    k.ySd = [[Dep() for _ in range(18)] for _ in range(NB)]
    k.outd = Dep()

    with ExitStack() as es:
        P = Prog(nc, es)
        k.P = P
        emit_all(k, es)
        P.finish()
    k.n_ins = P.n_ins
    return nc, k


def emit_consts(k, es):
    P = k.P
    k.identF = P.sb(es, "identF", [128, 128], F32)
    k.identB = P.sb(es, "identB", [128, 128], BF16)
    k.onesF = P.sb(es, "onesF", [128, 128], F32)
    k.onesB = P.sb(es, "onesB", [128, 128], BF16)
    P.memset("dve", k.onesF, k.onesF.ap, 1.0)
    P.memset("dve", k.onesB, k.onesB.ap, 1.0)
    P.fence_src = k.onesB.ap

    def sel(dst, pattern_step, base, cm, op=ALU.is_ge, src=None):
        src = src or k.onesF
        P.op("pool", lambda g: g.affine_select(out=dst.ap, in_=src.ap, pattern=[[pattern_step, 128]], compare_op=op,
                                               fill=0.0, base=base, channel_multiplier=cm),
             reads=[src], writes=[dst])

    sel(k.identF, 1, 0, -1, ALU.is_equal)
    P.copy("dve", k.identB.ap, k.identF.ap, [k.identF], [k.identB])
    k.m_le = P.sb(es, "m_le", [128, 128], F32)
    k.m_ge = P.sb(es, "m_ge", [128, 128], F32)
    k.m_gt = P.sb(es, "m_gt", [128, 128], F32)
    k.m_lt = P.sb(es, "m_lt", [128, 128], F32)
    sel(k.m_le, 1, 0, -1)
    sel(k.m_ge, -1, 0, 1)
    sel(k.m_gt, -1, -1, 1)
    sel(k.m_lt, 1, -1, -1)


def colvec(k, dst, dst_ap, src, src_ap, C, rows=128):
    P = k.P
    ps = P.psC()
    P.tr(ps, ps.ap[0:rows, 0:C], src_ap, k.identF, [src], np_=C)
    P.copy("dve", dst_ap, ps.ap[0:rows, 0:C], [ps], [dst])


def emit_load(k):
    P = k.P
    with ExitStack() as es:
        xin = [P.sb(es, f"xin{i}", [128, 4, D], F32) for i in range(2)]
        stg = [P.sb(es, f"stg{i}", [128, 8, 512], F32) for i in range(2)]
        it = 0
        for b in range(NB):
            for (src, tok0, nblk) in ((k.ctx, 0, CTX // 128), (k.x, CTX, SEQ // 128)):
                for g0 in range(0, nblk, 4):
                    nb = min(4, nblk - g0)
                    xi, sg = xin[it % 2], stg[it % 2]
                    it += 1
                    P.dma("sp", xi.ap[:, 0:nb, :],
                          src[b, g0 * 128:(g0 + nb) * 128, :].rearrange("(j p) f -> p j f", p=128), [], [xi])
                    for c in range(8):
                        ps = P.psA()
                        for j in range(nb):
                            P.tr(ps, ps.ap[:, j * 128:(j + 1) * 128], xi.ap[:, j, c * 128:(c + 1) * 128], k.identF,
                                 [xi], signal=(j == nb - 1))
                        P.copy("act" if c % 2 == 0 else "dve", sg.ap[:, c, 0:nb * 128], ps.ap[:, 0:nb * 128], [ps], [sg])
                    t0 = tok0 + g0 * 128
                    P.dma("act", k.hT[b, :, t0:t0 + nb * 128].rearrange("(c p) t -> p c t", p=128),
                          sg.ap[:, :, 0:nb * 128], [sg], hdeps(k.hTd, b, t0, nb * 128))
    P.barrier()


def emit_store(k):
    P = k.P
    with ExitStack() as es:
        hin = [P.sb(es, f"hin{i}", [128, 8, 512], F32) for i in range(2)]
        ost = [P.sb(es, f"ost{i}", [128, D], F32) for i in range(2)]
        it = 0
        io = 0
        for b in range(NB):
            for t0 in range(0, SEQ, 512):
                hi = hin[it % 2]
                it += 1
                P.dma("sp", hi.ap, k.hT[b, :, CTX + t0:CTX + t0 + 512].rearrange("(c p) t -> p c t", p=128),
                      hdeps(k.hTd, b, CTX + t0, 512), [hi])
                for j in range(4):
                    os_ = ost[io % 2]
                    io += 1
                    for half in range(2):
                        ps = P.psA()
                        for cc in range(4):
                            c = half * 4 + cc
                            P.tr(ps, ps.ap[:, cc * 128:(cc + 1) * 128], hi.ap[:, c, j * 128:(j + 1) * 128], k.identF,
                                 [hi], signal=(cc == 3))
                        P.copy("act" if half == 0 else "dve", os_.ap[:, half * 512:(half + 1) * 512], ps.ap, [ps], [os_])
                    P.dma("act", k.out[b, t0 + j * 128:t0 + (j + 1) * 128, :], os_.ap, [os_], [k.outd])
    P.barrier()


def emit_mod(k, es):
    P = k.P
    k.modT = P.sb(es, "modT", [128, 4, 48, 3], F32)
    k.gT = P.sb(es, "gT", [128, 4, 2, 8], F32)
    k.A = P.sb(es, "Amod", [128, 4, 2, 8, 3], F32)
    with ExitStack() as es2:
        sT = P.sb(es2, "sT", [128, 8, 3], F32)
        bm = P.sb(es2, "bm", [128, 4, 48], F32)
        wsl = [P.sb(es2, f"wsl{i}", [128, 8, 512], F32) for i in range(2)]
        cr = P.sb(es2, "cr", [8, 3, 128], F32)
        for b in range(NB):
            P.dma("sp", cr.ap[:, b, :], k.c[b, :].rearrange("(c p) -> c p", p=128), [], [cr])
        P.dma("sp", cr.ap[:, 2, :], k.w["c_ctx"].rearrange("(c p) -> c p", p=128), [], [cr])
        for r in range(3):
            colvec(k, sT, sT.ap[:, :, r], cr, cr.ap[:, r, :], 8)
        bmr = P.sb(es2, "bmr", [48, 4, 128], F32)
        P.dma("sp", bmr.ap, k.w["b_mod"].rearrange("i (j p) -> j i p", p=128), [], [bmr])
        gr = P.sb(es2, "gr", [8, 8, 128], F32)
        P.dma("sp", gr.ap, k.w["norm_g"].rearrange("i w (c p) -> c (i w) p", p=128), [], [gr])
        for i in range(4):
            colvec(k, bm, bm.ap[:, i, :], bmr, bmr.ap[:, i, :], 48)
            for w in range(2):
                colvec(k, k.gT, k.gT.ap[:, i, w, :], gr, gr.ap[:, i * 2 + w, :], 8)
        P.act(sT.ap, sT.ap, AF.Silu, [sT], [sT])
        it = 0
        for i in range(4):
            for s in range(12):
                ws = wsl[it % 2]
                it += 1
                P.dma("sp" if it % 2 == 0 else "act", ws.ap,
                      k.w["w_mod"][i, :, s * 512:(s + 1) * 512].rearrange("(c p) f -> p c f", p=128), [], [ws])
                ps = P.psC()
                for cc in range(4):
                    for kc in range(8):
                        P.mm(ps, ps.ap[:, cc * 3:(cc + 1) * 3], ws.ap[:, kc, cc * 128:(cc + 1) * 128], sT.ap[:, kc, :],
                             [ws, sT], kc == 0, kc == 7)
                P.tt("dve", k.modT.ap[:, i, s * 4:(s + 1) * 4, :],
                     ps.ap[:, 0:12].rearrange("p (a b) -> p a b", b=3),
                     bm.ap[:, i, s * 4:(s + 1) * 4].unsqueeze(2).to_broadcast([128, 4, 3]), ALU.add,
                     [ps, bm], [k.modT])
        for i in range(4):
            for w in range(2):
                kind = 1 + 3 * w
                P.op("dve", lambda g: g.scalar_tensor_tensor(
                    out=k.A.ap[:, i, w, :, :], in0=k.modT.ap[:, i, kind * 8:(kind + 1) * 8, :], scalar=1.0,
                    in1=k.gT.ap[:, i, w, :].unsqueeze(2).to_broadcast([128, 8, 3]), op0=ALU.add, op1=ALU.mult),
                    reads=[k.modT, k.gT], writes=[k.A])
    P.barrier()


def modulate_tile(k, i, w, r, h, n, sq, rs, u, u32=None):
    P = k.P
    for c in range(8):
        P.act(sq.ap[:, c, 0:n], h.ap[:, c, 0:n], AF.Square, [h], [sq])
    ps = P.psC()
    for c in range(8):
        P.mm(ps, ps.ap[:, 0:n], k.onesF.ap, sq.ap[:, c, 0:n], [k.onesF, sq], c == 0, c == 7)
    P.act(rs.ap[:, 0:n], ps.ap[:, 0:n], AF.Sqrt, [ps], [rs], bias=k.epsT.ap[:, 0:1], scale=1.0 / D)
    P.op("dve", lambda g: g.reciprocal(out=rs.ap[:, 0:n], in_=rs.ap[:, 0:n]), [rs], [rs])
    shift = (3 * w) * 8
    for c in range(8):
        P.tt("dve", sq.ap[:, c, 0:n], h.ap[:, c, 0:n], rs.ap[:, 0:n], ALU.mult, [h, rs], [sq])
        dst = u32 if u32 is not None else u
        P.ts("dve" if u32 is not None else "pool", dst.ap[:, c, 0:n], sq.ap[:, c, 0:n], k.A.ap[:, i, w, c, r:r + 1],
             k.modT.ap[:, i, shift + c, r:r + 1], ALU.mult, ALU.add, [sq, k.A, k.modT], [dst])
        if u32 is not None:
            P.copy("act", u.ap[:, c, 0:n], u32.ap[:, c, 0:n], [u32], [u])


def emit_modulate(k, i, last):
    P = k.P
    with ExitStack() as es:
        hb = [P.sb(es, f"mh{j}", [128, 8, 512], F32) for j in range(2)]
        sq = P.sb(es, "msq", [128, 8, 512], F32)
        rs = P.sb(es, "mrs", [128, 512], F32)
        ub = [P.sb(es, f"mu{j}", [128, 8, 512], BF16) for j in range(2)]
        for it, (b, r, t0, n) in enumerate(token_tiles()):
            h, u = hb[it % 2], ub[it % 2]
            P.dma("sp", h.ap[:, :, 0:n], k.hT[b, :, t0:t0 + n].rearrange("(c p) t -> p c t", p=128),
                  hdeps(k.hTd, b, t0, n), [h])
            modulate_tile(k, i, 0, r, h, n, sq, rs, u)
            P.dma("act", k.uT[b, :, t0:t0 + n].rearrange("(c p) t -> p c t", p=128), u.ap[:, :, 0:n], [u],
                  hdeps(k.uTd, b, t0, n))
    P.barrier()


def load_w_bf16(k, dst, dst_ap, src_ap):
    k.P.dma("pool", dst_ap, src_ap, [], [dst])


def emit_outproj(k, i, w_ap, KC, last, gscale=None):
    P = k.P
    with ExitStack() as es:
        wo = P.sb(es, "wo", [128, KC, D], BF16)
        for kc in range(0, KC, 4):
            n = min(4, KC - kc)
            load_w_bf16(k, wo, wo.ap[:, kc:kc + n, :], w_ap[kc * 128:(kc + n) * 128, :].rearrange("(c p) f -> p c f", p=128))
        ab = [P.sb(es, f"oa{j}", [128, KC, 512], BF16) for j in range(2)]
        hb = [P.sb(es, f"oh{j}", [128, 8, 512], F32) for j in range(2)]
        for it, (b, r, t0, n) in enumerate(token_tiles(with_ctx=not last)):
            a, h = ab[it % 2], hb[it % 2]
            P.dma("sp", a.ap[:, :, 0:n], k.aT[b, 0:KC * 128, t0:t0 + n].rearrange("(c p) t -> p c t", p=128),
                  hdeps(k.aTd, b, t0, n), [a])
            P.dma("sp", h.ap[:, :, 0:n], k.hT[b, :, t0:t0 + n].rearrange("(c p) t -> p c t", p=128),
                  hdeps(k.hTd, b, t0, n), [h])
            for oc in range(8):
                ps = P.psA()
                for kc in range(KC):
                    P.mm(ps, ps.ap[:, 0:n], wo.ap[:, kc, oc * 128:(oc + 1) * 128], a.ap[:, kc, 0:n], [wo, a],
                         kc == 0, kc == KC - 1)
                P.stt(h.ap[:, oc, 0:n], ps.ap[:, 0:n], k.modT.ap[:, i, 16 + oc, r:r + 1], h.ap[:, oc, 0:n],
                      ALU.mult, ALU.add, [ps, k.modT, h], [h])
            P.dma("act", k.hT[b, :, t0:t0 + n].rearrange("(c p) t -> p c t", p=128), h.ap[:, :, 0:n], [h],
                  hdeps(k.hTd, b, t0, n))
    P.barrier()


def super_tiles(last, width=768):
    groups = []
    for b in range(NB):
        tl = token_tiles((b,), with_ctx=not last, width=256)
        cur, curw = [], 0
        for t in tl:
            if curw + t[3] > width:
                groups.append(cur)
                cur, curw = [], 0
            cur.append(t)
            curw += t[3]
        if cur:
            groups.append(cur)
    out = []
    for g in groups:
        m = []
        for (b, r, t0, n) in g:
            if m and m[-1][1] == r and m[-1][2] + m[-1][3] == t0 and m[-1][3] + n <= 512:
                m[-1] = (b, r, m[-1][2], m[-1][3] + n)
            else:
                m.append((b, r, t0, n))
        out.append(m)
    return out


def emit_ffn(k, i, last, moe):
    P = k.P
    kk = i // 2
    W = 768
    PF = 2
    with ExitStack() as es:
        hres = P.sb(es, "f_h", [128, 8, W], F32)
        u = P.sb(es, "f_u", [128, 8, W], BF16)
        hid = P.sb(es, "f_hid", [128, 28, W], BF16)
        ring = [P.sb(es, f"f_w{j}", [128, 8192], BF16) for j in range(4)]
        sq = P.sb(es, "f_sq", [128, 8, 512], F32)
        u32 = P.sb(es, "f_u32", [128, 8, 512], F32) if moe else None
        rs = P.sb(es, "f_rs", [128, 512], F32)
        sg = [P.sb(es, f"f_sg{j}", [128, 512], F32) for j in range(2)]
        if moe:
            gbc = P.sb(es, "f_gbc", [128, 8, W], BF16)
            t2 = [P.sb(es, f"f_t2{j}", [128, 512], F32) for j in range(2)]
            wr = P.sb(es, "f_wr", [128, 8, 8], F32)
            P.dma("sp", wr.ap, k.w["moe_w_router"][kk].rearrange("(c p) e -> p c e", p=128), [], [wr])
            lg = P.sb(es, "f_lg", [128, 8], F32)
            mx = P.sb(es, "f_mx", [128, 8], F32)
            sm = P.sb(es, "f_sm", [128, 8], F32)
            ex = P.sb(es, "f_ex", [128, 8], F32)
            gt = P.sb(es, "f_gt", [128, 8], F32)
            gb = P.sb(es, "f_gb", [128, 8, 128], F32)
        groups = super_tiles(last, W)
        nexp = NE if moe else 1
        work = []
        for gi in range(len(groups)):
            for e in range(nexp):
                for j in range(7):
                    work.append((gi, e, "gu", j))
                for ds in range(4):
                    work.append((gi, e, "dn", ds))

        def wts(e):
            if moe:
                return k.w["moe_w_gate"][kk, e], k.w["moe_w_up"][kk, e], k.w["moe_w_down"][kk, e]
            return k.w["ffn_w_gate"][kk], k.w["ffn_w_up"][kk], k.w["ffn_w_down"][kk]

        def issue_load(wi):
            gi, e, kind, idx = work[wi]
            slot = ring[wi % 4]
            wg_, wu_, wd_ = wts(e)
            if kind == "gu":
                sv = slot.ap.rearrange("p (g c f) -> p g c f", g=2, c=8)
                load_w_bf16(k, slot, sv[:, 0], wg_[:, idx * 512:(idx + 1) * 512].rearrange("(c p) f -> p c f", p=128))
                load_w_bf16(k, slot, sv[:, 1], wu_[:, idx * 512:(idx + 1) * 512].rearrange("(c p) f -> p c f", p=128))
            else:
                sv = slot.ap[:, 0:28 * 256].rearrange("p (c f) -> p c f", c=28)
                for q4 in range(0, 28, 7):
                    load_w_bf16(k, slot, sv[:, q4:q4 + 7, :],
                                wd_[q4 * 128:(q4 + 7) * 128, idx * 256:(idx + 1) * 256].rearrange("(c p) f -> p c f", p=128))

        def prologue(grp, offs, b):
            for (_, r, t0, n), o in zip(grp, offs):
                hv = T(hres.ap[:, :, o:o + n], hres.dep)
                uv = T(u.ap[:, :, o:o + n], u.dep)
                P.dma("sp", hv.ap, k.hT[b, :, t0:t0 + n].rearrange("(c p) t -> p c t", p=128),
                      hdeps(k.hTd, b, t0, n), [hres])
                modulate_tile(k, i, 1, r, hv, n, sq, rs, uv, u32)
                if not moe:
                    continue
                for j in range(n // 128):
                    ps = P.psC()
                    for c in range(8):
                        P.mm(ps, ps.ap[:, 0:8], u32.ap[:, c, j * 128:(j + 1) * 128], wr.ap[:, c, :], [u32, wr],
                             c == 0, c == 7)
                    P.copy("dve", lg.ap, ps.ap[:, 0:8], [ps], [lg])
                    P.op("dve", lambda g: g.max(out=mx.ap, in_=lg.ap), [lg], [mx])
                    P.ts("dve", sm.ap, lg.ap, mx.ap[:, 1:2], None, ALU.is_ge, None, [lg, mx], [sm])
                    P.ts("dve", mx.ap[:, 2:3], mx.ap[:, 0:1], -1.0, None, ALU.mult, None, [mx], [mx])
                    P.act(ex.ap, lg.ap, AF.Exp, [lg, mx], [ex], bias=mx.ap[:, 2:3], scale=1.0)
                    P.act(mx.ap[:, 3:4], mx.ap[:, 1:2], AF.Exp, [mx], [mx], bias=mx.ap[:, 2:3], scale=1.0)
                    P.ts("dve", mx.ap[:, 3:4], mx.ap[:, 3:4], 1.0, None, ALU.add, None, [mx], [mx])
                    P.op("dve", lambda g: g.reciprocal(out=mx.ap[:, 4:5], in_=mx.ap[:, 3:4]), [mx], [mx])
                    P.stt(gt.ap, ex.ap, mx.ap[:, 4:5], sm.ap, ALU.mult, ALU.mult, [ex, mx, sm], [gt])
                    P.copy("dve", gb.ap, gt.ap.unsqueeze(2).to_broadcast([128, 8, 128]), [gt], [gb])
                    for e2 in range(0, 8, 4):
                        ps2 = P.psC()
                        for e in range(e2, e2 + 4):
                            P.mm(ps2, ps2.ap[:, (e - e2) * 128:(e - e2 + 1) * 128], gb.ap[:, e, :], k.identF.ap,
                                 [gb, k.identF], True, True)
                        P.copy("act", gbc.ap[:, e2:e2 + 4, o + j * 128:o + (j + 1) * 128],
                               ps2.ap.rearrange("p (e t) -> p e t", t=128), [ps2], [gbc])

        isg = 0
        for wi in range(min(PF, len(work))):
            issue_load(wi)
        for wi, (gi, e, kind, idx) in enumerate(work):
            if wi + PF < len(work):
                issue_load(wi + PF)
            grp = groups[gi]
            b = grp[0][0]
            offs = []
            o = 0
            for (_, r, t0, n) in grp:
                offs.append(o)
                o += n
            if e == 0 and kind == "gu" and idx == 0:
                prologue(grp, offs, b)
            slot = ring[wi % 4]
            if kind == "gu":
                j = idx
                sv = slot.ap.rearrange("p (g c f) -> p g c f", g=2, c=8)
                for hc in range(4):
                    for (_, r, t0, n), o in zip(grp, offs):
                        pg = P.psA()
                        for c in range(8):
                            P.mm(pg, pg.ap[:, 0:n], sv[:, 0, c, hc * 128:(hc + 1) * 128], u.ap[:, c, o:o + n],
                                 [slot, u], c == 0, c == 7)
                        pu = P.psA()
                        for c in range(8):
                            P.mm(pu, pu.ap[:, 0:n], sv[:, 1, c, hc * 128:(hc + 1) * 128], u.ap[:, c, o:o + n],
                                 [slot, u], c == 0, c == 7)
                        s_ = sg[isg % 2]
                        P.act(s_.ap[:, 0:n], pg.ap[:, 0:n], AF.Silu, [pg], [s_])
                        if moe:
                            t_ = t2[isg % 2]
                            P.tt("dve", t_.ap[:, 0:n], s_.ap[:, 0:n], pu.ap[:, 0:n], ALU.mult, [s_, pu], [t_])
                            P.tt("dve", hid.ap[:, j * 4 + hc, o:o + n], t_.ap[:, 0:n], gbc.ap[:, e, o:o + n],
                                 ALU.mult, [t_, gbc], [hid])
                        else:
                            P.tt("dve", hid.ap[:, j * 4 + hc, o:o + n], s_.ap[:, 0:n], pu.ap[:, 0:n], ALU.mult,
                                 [s_, pu], [hid])
                        isg += 1
            else:
                ds = idx
                sv = slot.ap[:, 0:28 * 256].rearrange("p (c f) -> p c f", c=28)
                for o2 in range(2):
                    oc = ds * 2 + o2
                    for (_, r, t0, n), o in zip(grp, offs):
                        pd = P.psB()
                        for c in range(28):
                            P.mm(pd, pd.ap[:, 0:n], sv[:, c, o2 * 128:(o2 + 1) * 128], hid.ap[:, c, o:o + n],
                                 [slot, hid], c == 0, c == 27)
                        P.stt(hres.ap[:, oc, o:o + n], pd.ap[:, 0:n], k.modT.ap[:, i, 40 + oc, r:r + 1],
                              hres.ap[:, oc, o:o + n], ALU.mult, ALU.add, [pd, k.modT, hres], [hres])
                if e == nexp - 1 and ds == 3:
                    for (_, r, t0, n), o in zip(grp, offs):
                        P.dma("act", k.hT[b, :, t0:t0 + n].rearrange("(c p) t -> p c t", p=128),
                              hres.ap[:, :, o:o + n], [hres], hdeps(k.hTd, b, t0, n))
    P.barrier()


def mixer_pool(k, i, last):
    P = k.P
    wins = (2, 4, 8, 16)
    with ExitStack() as es:
        win = P.sb(es, "p_win", [128, 8, D], BF16)
        for kc in range(0, 8, 4):
            load_w_bf16(k, win, win.ap[:, kc:kc + 4, :],
                        k.w["pool_w_in"][0, kc * 128:(kc + 4) * 128, :].rearrange("(c p) f -> p c f", p=128))
        wg = P.sb(es, "p_wg", [128, 4, 2, 256], BF16)
        for g in range(4):
            load_w_bf16(k, wg, wg.ap[:, g], k.w["pool_w_grp"][0, g].rearrange("(c p) o -> p c o", p=128))
        psc = P.sb(es, "p_psc", [128, 8], F32)
        pr = P.sb(es, "p_pr", [8, 128], F32)
        P.dma("sp", pr.ap, k.w["pool_scale"][0].rearrange("(c p) -> c p", p=128), [], [pr])
        colvec(k, psc, psc.ap, pr, pr.ap, 8)
        gs = P.sb(es, "p_gs", [128, 8, 3], F32)
        P.tt("dve", gs.ap, k.modT.ap[:, i, 16:24, :], psc.ap.unsqueeze(2).to_broadcast([128, 8, 3]), ALU.mult,
             [k.modT, psc], [gs])
        rcL = P.sb(es, "p_rcL", [128, 4, SEQ], F32)
        rcC = P.sb(es, "p_rcC", [128, 4, CTX], F32)
        with ExitStack() as es2:
            ti = P.sb(es2, "p_ti", [128, SEQ], I32)
            tf = P.sb(es2, "p_tf", [128, SEQ], F32)
            hi = P.sb(es2, "p_hi", [128, SEQ], F32)
            P.op("pool", lambda g: g.iota(out=ti.ap, pattern=[[1, SEQ]], base=0, channel_multiplier=0), [], [ti])
            P.copy("dve", tf.ap, ti.ap, [ti], [tf])
            for (rc, n) in ((rcL, SEQ), (rcC, CTX)):
                for wi_, w in enumerate(wins):
                    P.ts("dve", hi.ap[:, 0:n], tf.ap[:, 0:n], float(w // 2), float(n), ALU.add, ALU.min, [tf], [hi])
                    P.ts("dve", rc.ap[:, wi_, :], tf.ap[:, 0:n], float(-(w // 2)), 0.0, ALU.add, ALU.max, [tf], [rc])
                    P.tt("dve", rc.ap[:, wi_, :], hi.ap[:, 0:n], rc.ap[:, wi_, :], ALU.subtract, [hi, rc], [rc])
                    P.op("dve", lambda g: g.reciprocal(out=rc.ap[:, wi_, :], in_=rc.ap[:, wi_, :]), [rc], [rc])
        P.barrier()
        us = P.sb(es, "p_us", [128, 8, SEQ], BF16)
        vps = [P.sb(es, f"p_vp{j}", [128, SEQ + 16], F32) for j in range(2)]
        sA = P.sb(es, "p_sA", [128, SEQ + 16], F32)
        sB = P.sb(es, "p_sB", [128, SEQ + 16], F32)
        tmp = P.sb(es, "p_tmp", [128, SEQ], F32)
        pl = P.sb(es, "p_pl", [128, 8, SEQ], BF16)
        hb = [P.sb(es, f"p_h{j}", [128, 8, 512], F32) for j in range(2)]
        ih = 0
        iv = 0
        for b in range(NB):
            segs = [(CTX, SEQ, b, rcL)] if last else [(0, CTX, 2, rcC), (CTX, SEQ, b, rcL)]
            for (tok0, n, r, rc) in segs:
                P.dma("sp", us.ap[:, :, 0:n], k.uT[b, :, tok0:tok0 + n].rearrange("(c p) t -> p c t", p=128),
                      hdeps(k.uTd, b, tok0, n), [us])
                for fc in range(8):
                    g = fc // 2
                    w = wins[g]
                    vp = vps[iv % 2]
                    iv += 1
                    P.memset("pool", vp, vp.ap[:, 0:8], 0.0)
                    P.memset("pool", vp, vp.ap[:, 8 + n:16 + n], 0.0)
                    for t0 in range(0, n, 512):
                        N = min(512, n - t0)
                        ps = P.psA()
                        for kc in range(8):
                            P.mm(ps, ps.ap[:, 0:N], win.ap[:, kc, fc * 128:(fc + 1) * 128], us.ap[:, kc, t0:t0 + N],
                                 [win, us], kc == 0, kc == 7)
                        P.copy("act", vp.ap[:, 8 + t0:8 + t0 + N], ps.ap[:, 0:N], [ps], [vp])
                    S, L, step, nb_ = vp, n + 16, 1, 0
                    while 2 * step <= w:
                        nxt = sA if nb_ % 2 == 0 else sB
                        nb_ += 1
                        L = L - step
                        P.tt("dve", nxt.ap[:, 0:L], S.ap[:, 0:L], S.ap[:, step:step + L], ALU.add, [S], [nxt])
                        S = nxt
                        step *= 2
                    o = 8 - w // 2
                    P.tt(k.pool_eng, tmp.ap[:, 0:n], S.ap[:, o:o + n], rc.ap[:, g, 0:n], ALU.mult, [S, rc], [tmp])
                    P.tt("dve", pl.ap[:, fc, 0:n], tmp.ap[:, 0:n], vp.ap[:, 8:8 + n], ALU.subtract, [tmp, vp], [pl])
                    if k.dbg_barrier:
                        P.barrier()
                for t0 in range(0, n, 512):
                    N = min(512, n - t0)
                    h = hb[ih % 2]
                    ih += 1
                    P.dma("sp", h.ap[:, :, 0:N], k.hT[b, :, tok0 + t0:tok0 + t0 + N].rearrange("(c p) t -> p c t", p=128),
                          hdeps(k.hTd, b, tok0 + t0, N), [h])
                    for fo in range(8):
                        g, oc = fo // 2, fo % 2
                        ps = P.psB()
                        for ic in range(2):
                            P.mm(ps, ps.ap[:, 0:N], wg.ap[:, g, ic, oc * 128:(oc + 1) * 128],
                                 pl.ap[:, 2 * g + ic, t0:t0 + N], [wg, pl], ic == 0, ic == 1)
                        P.stt(h.ap[:, fo, 0:N], ps.ap[:, 0:N], gs.ap[:, fo, r:r + 1], h.ap[:, fo, 0:N], ALU.mult, ALU.add,
                              [ps, gs, h], [h])
                    P.dma("act", k.hT[b, :, tok0 + t0:tok0 + t0 + N].rearrange("(c p) t -> p c t", p=128), h.ap[:, :, 0:N],
                          [h], hdeps(k.hTd, b, tok0 + t0, N))
    P.barrier()


def mixer_lru(k, i, last):
    P = k.P
    LW = 1280
    with ExitStack() as es:
        win = P.sb(es, "l_win", [128, 8, 2 * LW], BF16)
        for s5 in range(5):
            load_w_bf16(k, win, win.ap[:, :, s5 * 512:(s5 + 1) * 512],
                        k.w["lru_w_in"][0, :, s5 * 512:(s5 + 1) * 512].rearrange("(c p) f -> p c f", p=128))
        gw = P.sb(es, "l_gw", [128, 2, 2, 10, 128], BF16)
        for d in range(2):
            for j in range(2):
                load_w_bf16(k, gw, gw.ap[:, d, j], k.w["lru_gate_w"][0, d, j].rearrange("c p o -> p c o"))
        vecs = P.sb(es, "l_vecs", [128, 110], F32)
        vst = P.sb(es, "l_vst", [110, 128], F32)
        P.dma("sp", vst.ap[0:40, :], k.w["lru_conv_w"][0].rearrange("j (c p) -> (j c) p", p=128), [], [vst])
        P.dma("sp", vst.ap[40:50, :], k.w["lru_conv_b"][0].rearrange("(c p) -> c p", p=128), [], [vst])
        P.dma("sp", vst.ap[50:90, :], k.w["lru_gate_b"][0].rearrange("d j (c p) -> (d j c) p", p=128), [], [vst])
        P.dma("sp", vst.ap[90:110, :], k.w["lru_lambda"][0].rearrange("d (c p) -> (d c) p", p=128), [], [vst])
        colvec(k, vecs, vecs.ap, vst, vst.ap, 110)
        cw = vecs.ap[:, 0:40].rearrange("p (j c) -> p j c", j=4)
        cb = vecs.ap[:, 40:50]
        gbias = vecs.ap[:, 50:90].rearrange("p (d j c) -> p d j c", d=2, j=2)
        m8 = P.sb(es, "l_m8", [128, 20], F32)
        P.act(m8.ap, vecs.ap[:, 90:110], AF.Exp, [vecs], [m8], scale=-1.0)
        P.act(m8.ap, m8.ap, AF.Ln, [m8], [m8], bias=k.oneT.ap[:, 0:1], scale=1.0)
        P.ts("dve", m8.ap, m8.ap, -8.0, None, ALU.mult, None, [m8], [m8])
        u = P.sb(es, "l_u", [128, 8, TOK], BF16)
        XPW = 2312
        xp = P.sb(es, "l_xp", [128, XPW], F32)
        xr = P.sb(es, "l_xr", [128, TOK], F32)
        xrb = P.sb(es, "l_xrb", [128, TOK], BF16)
        gx = P.sb(es, "l_gx", [128, TOK], F32)
        A = P.sb(es, "l_A", [128, TOK], F32)
        Bx = P.sb(es, "l_Bx", [128, TOK], F32)
        tmp = P.sb(es, "l_tmp", [128, TOK], F32)
        hf = P.sb(es, "l_hf", [128, TOK], F32)
        hbk = P.sb(es, "l_hb", [128, TOK], F32)
        yb = P.sb(es, "l_yb", [128, TOK], BF16)
        P.memset("pool", xp, xp.ap, 0.0)
        segs = [(0, CTX, 2), (CTX, SEQ, 262)]
        tiles = [(0, CTX, 2)] + [(CTX + t, 512, 262 + t) for t in range(0, SEQ, 512)]
        for b in range(NB):
            P.dma("sp", u.ap, k.uT[b].rearrange("(c p) t -> p c t", p=128), hdeps(k.uTd, b, 0, TOK), [u])
            for ch in range(10):
                for (t0, N, xc) in tiles:
                    ps = P.psA()
                    for kc in range(8):
                        P.mm(ps, ps.ap[:, 0:N], win.ap[:, kc, LW + ch * 128:LW + (ch + 1) * 128], u.ap[:, kc, t0:t0 + N],
                             [win, u], kc == 0, kc == 7)
                    P.copy("act", xp.ap[:, xc:xc + N], ps.ap[:, 0:N], [ps], [xp])
                for (t0, n, s0) in segs:
                    P.ts("dve", xr.ap[:, t0:t0 + n], xp.ap[:, s0 - 2:s0 - 2 + n], cw[:, 0, ch:ch + 1], cb[:, ch:ch + 1],
                         ALU.mult, ALU.add, [xp, vecs], [xr])
                    for j in range(1, 4):
                        P.stt(xr.ap[:, t0:t0 + n], xp.ap[:, s0 - 2 + j:s0 - 2 + j + n], cw[:, j, ch:ch + 1],
                              xr.ap[:, t0:t0 + n], ALU.mult, ALU.add, [xp, vecs, xr], [xr])
                P.copy("act", xrb.ap, xr.ap, [xr], [xrb])
                for (t0, N, xc) in tiles:
                    ps = P.psA()
                    for kc in range(8):
                        P.mm(ps, ps.ap[:, 0:N], win.ap[:, kc, ch * 128:(ch + 1) * 128], u.ap[:, kc, t0:t0 + N],
                             [win, u], kc == 0, kc == 7)
                    P.copy("act", gx.ap[:, t0:t0 + N], ps.ap[:, 0:N], [ps], [gx])
                P.act(tmp.ap, gx.ap, AF.Square, [gx], [tmp])
                P.ts("dve", tmp.ap, tmp.ap, 0.044715, 1.0, ALU.mult, ALU.add, [tmp], [tmp])
                P.tt("dve", tmp.ap, tmp.ap, gx.ap, ALU.mult, [tmp, gx], [tmp])
                P.act(tmp.ap, tmp.ap, AF.Sigmoid, [tmp], [tmp], scale=1.5957691216057308)
                P.tt("dve", gx.ap, gx.ap, tmp.ap, ALU.mult, [gx, tmp], [gx])
                for d in range(2):
                    for j, dst in ((0, A), (1, Bx)):
                        for (t0, N, xc) in tiles:
                            ps = P.psB()
                            P.mm(ps, ps.ap[:, 0:N], gw.ap[:, d, j, ch, :], xrb.ap[:, t0:t0 + N], [gw, xrb], True, True)
                            P.act(dst.ap[:, t0:t0 + N], ps.ap[:, 0:N], AF.Sigmoid, [ps, vecs], [dst],
                                  bias=gbias[:, d, j, ch:ch + 1], scale=1.0)
                    P.act(A.ap, A.ap, AF.Exp, [A, m8], [A], scale=m8.ap[:, d * 10 + ch:d * 10 + ch + 1])
                    P.act(tmp.ap, A.ap, AF.Square, [A], [tmp])
                    P.act(tmp.ap, tmp.ap, AF.Sqrt, [tmp], [tmp], bias=k.oneT.ap[:, 0:1], scale=-1.0)
                    P.tt("dve", Bx.ap, Bx.ap, xr.ap, ALU.mult, [Bx, xr], [Bx])
                    P.tt("dve", Bx.ap, Bx.ap, tmp.ap, ALU.mult, [Bx, tmp], [Bx])
                    if d == 0:
                        P.op("dve", lambda g: g.tensor_tensor_scan(out=hf.ap, data0=A.ap, data1=Bx.ap, initial=0.0,
                                                                   op0=ALU.mult, op1=ALU.add), [A, Bx], [hf])
                    else:
                        P.op("dve", lambda g: g.tensor_tensor_scan(
                            out=hbk.ap[:, 0:CTX][:, ::-1], data0=A.ap[:, 0:CTX][:, ::-1], data1=Bx.ap[:, 0:CTX][:, ::-1],
                            initial=0.0, op0=ALU.mult, op1=ALU.add), [A, Bx], [hbk])
                        P.op("dve", lambda g: g.tensor_tensor_scan(
                            out=hbk.ap[:, CTX:TOK][:, ::-1], data0=A.ap[:, CTX:TOK][:, ::-1],
                            data1=Bx.ap[:, CTX:TOK][:, ::-1], initial=hbk.ap[:, 0:1], op0=ALU.mult, op1=ALU.add),
                            [A, Bx, hbk], [hbk])
                P.tt("dve", hf.ap, hf.ap, hbk.ap, ALU.add, [hf, hbk], [hf])
                P.tt("dve", yb.ap, hf.ap, gx.ap, ALU.mult, [hf, gx], [yb])
                P.dma("act", k.aT[b, ch * 128:(ch + 1) * 128, :], yb.ap, [yb], hdeps(k.aTd, b, 0, TOK))
    P.barrier()
    emit_outproj(k, i, k.w["lru_w_out"][0], 10, last)


def mixer_gqa(k, i, last):
    P = k.P
    HD = 64
    with ExitStack() as es:
        W = P.sb(es, "a_w", [128, 8, 1536], BF16)
        for s3 in range(3):
            load_w_bf16(k, W, W.ap[:, :, s3 * 512:(s3 + 1) * 512],
                        k.w["att_w_qkv"][0, :, s3 * 512:(s3 + 1) * 512].rearrange("(c p) f -> p c f", p=128))
        gst = P.sb(es, "a_gst", [2, HD], F32)
        P.dma("sp", gst.ap[0:1, :], k.w["att_q_norm"][0:1, :], [], [gst])
        P.dma("sp", gst.ap[1:2, :], k.w["att_k_norm"][0:1, :], [], [gst])
        gqk = P.sb(es, "a_gqk", [HD, 2], F32)
        colvec(k, gqk, gqk.ap, gst, gst.ap, 2, rows=HD)
        cos = P.sb(es, "a_cos", [HD, SEQ], F32)
        sin = P.sb(es, "a_sin", [HD, SEQ], F32)
        P.dma("sp", cos.ap, k.rope_cos, [], [cos])
        P.dma("sp", sin.ap, k.rope_sin, [], [sin])
        pm = P.sb(es, "a_pm", [HD, HD], F32)
        pm2 = P.sb(es, "a_pm2", [HD, HD], F32)
        P.op("pool", lambda g: g.affine_select(out=pm.ap, in_=k.onesF.ap[0:HD, 0:HD], pattern=[[-1, HD]],
                                               compare_op=ALU.is_equal, fill=0.0, base=32, channel_multiplier=1),
             [k.onesF], [pm])
        P.op("pool", lambda g: g.affine_select(out=pm2.ap, in_=k.onesF.ap[0:HD, 0:HD], pattern=[[-1, HD]],
                                               compare_op=ALU.is_equal, fill=0.0, base=-32, channel_multiplier=1),
             [k.onesF], [pm2])
        P.tt("dve", pm.ap, pm.ap, pm2.ap, ALU.subtract, [pm, pm2], [pm])
        u = P.sb(es, "a_u", [128, 8, TOK], BF16)
        raw = P.sb(es, "a_raw", [HD, TOK], F32)
        sq = P.sb(es, "a_sq", [HD, TOK], F32)
        rsd = P.sb(es, "a_rsd", [HD, TOK], F32)
        hat = P.sb(es, "a_hat", [HD, TOK], F32)
        KT = P.sb(es, "a_KT", [128, TOK], BF16)
        QT = [P.sb(es, f"a_QT{j}", [128, TOK], BF16) for j in range(2)]
        for t_ in [KT] + QT:
            P.memset("dve", t_, t_.ap, 0.0)
        V = P.sb(es, "a_V", [128, 18, HD], BF16)
        t1 = P.sb(es, "a_t1", [HD, 512], F32)
        t2 = P.sb(es, "a_t2", [HD, 512], F32)
        pts = [P.sb(es, f"a_pt{j}", [128, 512], BF16) for j in range(3)]
        rden = P.sb(es, "a_rden", [HD, 512], F32)
        ots = [P.sb(es, f"a_ot{j}", [HD, 512], BF16) for j in range(2)]
        tiles = [(0, CTX)] + [(CTX + t, 512) for t in range(0, SEQ, 512)]
        ipt = 0
        iq = 0

        def project(col0, gain_col, dst):
            for (t0, N) in tiles:
                ps = P.psA()
                for kc in range(8):
                    P.mm(ps, ps.ap[0:HD, 0:N], W.ap[:, kc, col0:col0 + HD], u.ap[:, kc, t0:t0 + N], [W, u], kc == 0, kc == 7)
                P.copy("act", raw.ap[:, t0:t0 + N], ps.ap[0:HD, 0:N], [ps], [raw])
            P.act(sq.ap, raw.ap, AF.Square, [raw], [sq])
            for (t0, N) in tiles:
                ps = P.psA()
                P.mm(ps, ps.ap[0:HD, 0:N], k.onesF.ap[0:HD, 0:HD], sq.ap[:, t0:t0 + N], [k.onesF, sq], True, True)
                P.act(rsd.ap[:, t0:t0 + N], ps.ap[0:HD, 0:N], AF.Sqrt, [ps], [rsd], bias=k.epsT.ap[0:HD, 0:1], scale=1.0 / HD)
            P.op("dve", lambda g: g.reciprocal(out=rsd.ap, in_=rsd.ap), [rsd], [rsd])
            P.stt(hat.ap, raw.ap, gqk.ap[:, gain_col:gain_col + 1], rsd.ap, ALU.mult, ALU.mult, [raw, gqk, rsd], [hat])
            P.copy("act", dst.ap[0:HD, 0:CTX], hat.ap[:, 0:CTX], [hat], [dst])
            for t in range(0, SEQ, 512):
                ps = P.psA()
                P.mm(ps, ps.ap[0:HD, 0:512], pm.ap, hat.ap[:, CTX + t:CTX + t + 512], [pm, hat], True, True)
                P.tt("dve", t1.ap, hat.ap[:, CTX + t:CTX + t + 512], cos.ap[:, t:t + 512], ALU.mult, [hat, cos], [t1])
                P.tt("dve", t2.ap, ps.ap[0:HD, 0:512], sin.ap[:, t:t + 512], ALU.mult, [ps, sin], [t2])
                P.tt("dve", dst.ap[0:HD, CTX + t:CTX + t + 512], t1.ap, t2.ap, ALU.add, [t1, t2], [dst])

        for b in range(NB):
            P.dma("sp", u.ap, k.uT[b].rearrange("(c p) t -> p c t", p=128), hdeps(k.uTd, b, 0, TOK), [u])
            for g in range(4):
                project(1024 + g * HD, 1, KT)
                for b0 in range(0, 18, 8):
                    nb = min(8, 18 - b0)
                    ps = P.psA()
                    for j in range(nb):
                        for kc in range(8):
                            P.mm(ps, ps.ap[:, j * HD:(j + 1) * HD], u.ap[:, kc, (b0 + j) * 128:(b0 + j + 1) * 128],
                                 W.ap[:, kc, 1280 + g * HD:1280 + (g + 1) * HD], [W, u], kc == 0, kc == 7)
                    P.copy("act", V.ap[:, b0:b0 + nb, :], ps.ap[:, 0:nb * HD].rearrange("p (j d) -> p j d", d=HD), [ps], [V])
                for r in range(4):
                    h = g * 4 + r
                    Q = QT[h % 2]
                    project(h * HD, 0, Q)
                    qtiles = [(CTX + t, 512, 0, 18) for t in range(0, SEQ, 512)]
                    if not last:
                        qtiles.append((0, CTX, 0, 2))
                    for (q0, N, kb0, kb1) in qtiles:
                        po = P.ps[4 + 2 * (iq % 2)]
                        pd = P.ps[5 + 2 * (iq % 2)]
                        ot = ots[iq % 2]
                        iq += 1
                        pSs = {}

                        def issue_s(kb_):
                            pS_ = P.psA()
                            P.mm(pS_, pS_.ap[:, 0:N], KT.ap[:, kb_ * 128:(kb_ + 1) * 128], Q.ap[:, q0:q0 + N], [KT, Q], True, True)
                            pSs[kb_] = pS_

                        issue_s(kb0)
                        for kb in range(kb0, kb1):
                            if kb + 1 < kb1:
                                issue_s(kb + 1)
                            pS = pSs.pop(kb)
                            pt = pts[ipt % 3]
                            ipt += 1
                            P.act(pt.ap[:, 0:N], pS.ap[:, 0:N], AF.Exp, [pS], [pt], scale=0.125)
                            P.mm(po, po.ap[0:HD, 0:N], V.ap[:, kb, :], pt.ap[:, 0:N], [V, pt], kb == kb0, kb == kb1 - 1)
                            P.mm(pd, pd.ap[0:HD, 0:N], k.onesB.ap[:, 0:HD], pt.ap[:, 0:N], [k.onesB, pt], kb == kb0, kb == kb1 - 1)
                        P.op("dve", lambda g_: g_.reciprocal(out=rden.ap[:, 0:N], in_=pd.ap[0:HD, 0:N]), [pd], [rden])
                        P.tt("dve", ot.ap[:, 0:N], po.ap[0:HD, 0:N], rden.ap[:, 0:N], ALU.mult, [po, rden], [ot])
                        P.dma("act", k.aT[b, h * HD:(h + 1) * HD, q0:q0 + N], ot.ap[:, 0:N], [ot], hdeps(k.aTd, b, q0, N))
    P.barrier()
    emit_outproj(k, i, k.w["att_w_out"][0], 8, last)


def bcast_rows(k, dst, dst_ap, row, row_ap, n):
    P = k.P
    for c0 in range(0, n, 512):
        m = min(512, n - c0)
        ps = P.psC()
        P.mm(ps, ps.ap[:, 0:m], k.onesF.ap[0:1, :], row_ap[:, c0:c0 + m], [k.onesF, row], True, True)
        P.copy("dve", dst_ap[:, c0:c0 + m], ps.ap[:, 0:m], [ps], [dst])


def mixer_ssd(k, i, last):
    P = k.P
    Win = k.w["ssm_w_in"][0]
    XPW = 2312
    with ExitStack() as es:
        Wdt = P.sb(es, "s_wdt", [128, 8, 64], BF16)
        load_w_bf16(k, Wdt, Wdt.ap, Win[:, 5120:5184].rearrange("(c p) f -> p c f", p=128))
        cst = P.sb(es, "s_cst", [120, 128], F32)
        P.dma("sp", cst.ap[0:96, :], k.w["ssm_conv_w"][0].rearrange("j (c p) -> (j c) p", p=128), [], [cst])
        P.dma("sp", cst.ap[96:120, :], k.w["ssm_conv_b"][0].rearrange("(c p) -> c p", p=128), [], [cst])
        cvec = P.sb(es, "s_cvec", [128, 120], F32)
        colvec(k, cvec, cvec.ap, cst, cst.ap, 120)
        cw = cvec.ap[:, 0:96].rearrange("p (j c) -> p j c", j=4)
        cb = cvec.ap[:, 96:120]
        row = P.sb(es, "s_row", [1, 160 + 2048], F32)
        P.dma("sp", row.ap[:, 0:64], k.w["ssm_dt_bias"][0:1].rearrange("o d h -> o (d h)"), [], [row])
        P.dma("sp", row.ap[:, 64:128], k.w["ssm_a_log"][0:1].rearrange("o d h -> o (d h)"), [], [row])
        P.dma("sp", row.ap[:, 128:160], k.w["ssm_d"][0:1, :], [], [row])
        P.dma("sp", row.ap[:, 160:160 + 2048], k.w["ssm_norm_g"][0:1, :], [], [row])
        rep = P.sb(es, "s_rep", [128, 160], F32)
        bcast_rows(k, rep, rep.ap, row, row.ap[:, 0:160], 160)
        P.act(rep.ap[:, 64:128], rep.ap[:, 64:128], AF.Exp, [rep], [rep])
        P.ts("dve", rep.ap[:, 64:128], rep.ap[:, 64:128], -1.0, None, ALU.mult, None, [rep], [rep])
        ngR = P.sb(es, "s_ng", [128, 2048], F32)
        bcast_rows(k, ngR, ngR.ap, row, row.ap[:, 160:160 + 2048], 2048)
        ssq = P.sb(es, "s_ssq", [128, NB, 18], F32)
        dtbR = rep.ap[:, 0:64].rearrange("p (d h) -> p d h", d=2)
        negR = rep.ap[:, 64:128].rearrange("p (d h) -> p d h", d=2)
        tiles = [(0, CTX, 2)] + [(CTX + t, 512, 262 + t) for t in range(0, SEQ, 512)]
        segs = [(0, CTX, 2), (CTX, SEQ, 262)]
        with ExitStack() as esg:
            Wg = P.sb(esg, "s_wg", [128, 8, 1280], BF16)
            xs_tm = P.sb(esg, "s_xs", [128, 18, 512], F32)
            BT = P.sb(esg, "s_BT", [128, TOK], BF16)
            CT = P.sb(esg, "s_CT", [128, TOK], BF16)
            Btm = P.sb(esg, "s_Btm", [128, 18, 128], BF16)
            zs = P.sb(esg, "s_zs", [128, 18, 512], BF16)
            dt = P.sb(esg, "s_dt", [128, 18, 2, 8], F32)
            la = P.sb(esg, "s_la", [128, 18, 2, 8], F32)
            for g in range(4):
                load_w_bf16(k, Wg, Wg.ap[:, :, 0:512], Win[:, g * 512:(g + 1) * 512].rearrange("(c p) f -> p c f", p=128))
                load_w_bf16(k, Wg, Wg.ap[:, :, 512:1024],
                            Win[:, 2048 + g * 512:2048 + (g + 1) * 512].rearrange("(c p) f -> p c f", p=128))
                load_w_bf16(k, Wg, Wg.ap[:, :, 1024:1152],
                            Win[:, 4096 + g * 128:4096 + (g + 1) * 128].rearrange("(c p) f -> p c f", p=128))
                load_w_bf16(k, Wg, Wg.ap[:, :, 1152:1280],
                            Win[:, 4608 + g * 128:4608 + (g + 1) * 128].rearrange("(c p) f -> p c f", p=128))
                for b in range(NB):
                    with ExitStack() as ea:
                        u = P.sb(ea, "s_u", [128, 8, TOK], BF16)
                        xps = [P.sb(ea, f"s_xp{j}", [128, XPW], F32) for j in range(2)]
                        cv = P.sb(ea, "s_cv", [128, TOK], F32)
                        sTs = [P.sb(ea, f"s_sT{j}", [128, TOK], F32) for j in range(2)]
                        for xp in xps:
                            P.memset("pool", xp, xp.ap, 0.0)
                        P.dma("sp", u.ap, k.uT[b].rearrange("(c p) t -> p c t", p=128), hdeps(k.uTd, b, 0, TOK), [u])
                        jobs = [(512 + c * 128, g * 4 + c, "x", c) for c in range(4)] + [(1024, 16 + g, "B", 0), (1152, 20 + g, "C", 0)]
                        for ji, (wcol, cch, kind, c) in enumerate(jobs):
                            xp = xps[ji % 2]
                            sT = sTs[ji % 2]
                            for (t0, N, xc) in tiles:
                                ps = P.psA()
                                for kc in range(8):
                                    P.mm(ps, ps.ap[:, 0:N], Wg.ap[:, kc, wcol:wcol + 128], u.ap[:, kc, t0:t0 + N], [Wg, u],
                                         kc == 0, kc == 7)
                                P.copy("act", xp.ap[:, xc:xc + N], ps.ap[:, 0:N], [ps], [xp])
                            for (t0, n, s0) in segs:
                                P.ts("dve", cv.ap[:, t0:t0 + n], xp.ap[:, s0 - 2:s0 - 2 + n], cw[:, 0, cch:cch + 1],
                                     cb[:, cch:cch + 1], ALU.mult, ALU.add, [xp, cvec], [cv])
                                for j in range(1, 4):
                                    P.stt(cv.ap[:, t0:t0 + n], xp.ap[:, s0 - 2 + j:s0 - 2 + j + n], cw[:, j, cch:cch + 1],
                                          cv.ap[:, t0:t0 + n], ALU.mult, ALU.add, [xp, cvec, cv], [cv])
                            if kind == "C":
                                P.act(CT.ap, cv.ap, AF.Silu, [cv], [CT])
                                continue
                            P.act(sT.ap, cv.ap, AF.Silu, [cv], [sT])
                            if kind == "B":
                                P.copy("dve", BT.ap, sT.ap, [sT], [BT])
                            for b0 in range(0, 18, 4):
                                nb = min(4, 18 - b0)
                                ps = P.psB()
                                for j in range(nb):
                                    P.tr(ps, ps.ap[:, j * 128:(j + 1) * 128], sT.ap[:, (b0 + j) * 128:(b0 + j + 1) * 128],
                                         k.identF, [sT], signal=(j == nb - 1))
                                src = ps.ap[:, 0:nb * 128].rearrange("p (j f) -> p j f", f=128)
                                if kind == "x":
                                    P.copy("act" if (b0 // 4) % 2 == 0 else "dve", xs_tm.ap[:, b0:b0 + nb, c * 128:(c + 1) * 128],
                                           src, [ps], [xs_tm])
                                else:
                                    P.copy("dve", Btm.ap[:, b0:b0 + nb, :], src, [ps], [Btm])
                        for blk in range(18):
                            ps = P.psA()
                            for kc in range(8):
                                P.mm(ps, ps.ap, u.ap[:, kc, blk * 128:(blk + 1) * 128], Wg.ap[:, kc, 0:512], [Wg, u],
                                     kc == 0, kc == 7)
                            P.act(zs.ap[:, blk, :], ps.ap, AF.Silu, [ps], [zs])
                        psd = P.psC()
                        for blk in range(18):
                            for d in range(2):
                                for kc in range(8):
                                    P.mm(psd, psd.ap[:, blk * 16 + d * 8:blk * 16 + d * 8 + 8], u.ap[:, kc, blk * 128:(blk + 1) * 128],
                                         Wdt.ap[:, kc, d * 32 + g * 8:d * 32 + g * 8 + 8], [Wdt, u], kc == 0, kc == 7)
                        P.tt("dve", dt.ap, psd.ap[:, 0:288].rearrange("p (b d r) -> p b d r", d=2, r=8),
                             dtbR[:, :, g * 8:(g + 1) * 8].unsqueeze(1).to_broadcast([128, 18, 2, 8]), ALU.add, [psd, rep], [dt])
                        P.ts("dve", la.ap, dt.ap, -1.0, None, ALU.mult, None, [dt], [la])
                        P.tt("dve", la.ap, la.ap, dt.ap, ALU.min, [la, dt], [la])
                        P.act(la.ap, la.ap, AF.Exp, [la], [la])
                        P.act(la.ap, la.ap, AF.Ln, [la], [la], bias=k.oneT.ap[:, 0:1], scale=1.0)
                        P.stt(dt.ap, dt.ap, 0.0, la.ap, ALU.max, ALU.add, [dt, la], [dt])
                        P.tt("dve", la.ap, dt.ap, negR[:, :, g * 8:(g + 1) * 8].unsqueeze(1).to_broadcast([128, 18, 2, 8]),
                             ALU.mult, [dt, rep], [la])
                        if k.debug and g == 1 and b == 1:
                            P.dma("act", k.dbg[:, 0:288], dt.ap.rearrange("p a d r -> p (a d r)"), [dt], [k.dbgd])
                            P.dma("act", k.dbg[:, 288:576], la.ap.rearrange("p a d r -> p (a d r)"), [la], [k.dbgd])
                    P.barrier()
                    with ExitStack() as eb:
                        yacc = P.sb(eb, "s_yacc", [128, 18, 512], F32)
                        H = P.sb(eb, "s_H", [128, 8, 64], F32)
                        Hb = P.sb(eb, "s_Hb", [128, 512], BF16)
                        xdts = [P.sb(eb, f"s_xdt{j}", [128, 8, 64], BF16) for j in range(2)]
                        xws = [P.sb(eb, f"s_xw{j}", [128, 8, 64], BF16) for j in range(2)]
                        cbms = [P.sb(eb, f"s_cbm{j}", [128, 128], F32) for j in range(2)]
                        E3s = [P.sb(eb, f"s_E3{j}", [128, 24], F32) for j in range(2)]
                        rDs = [P.sb(eb, f"s_rD{j}", [128, 4, 128], F32) for j in range(2)]
                        EDs = [P.sb(eb, f"s_ED{j}", [128, 4, 128], F32) for j in range(2)]
                        MTs = [P.sb(eb, f"s_MT{j}", [128, 8, 128], BF16) for j in range(2)]
                        tmpo = [P.sb(eb, f"s_to{j}", [128, 8, 64], F32) for j in range(2)]
                        it = 0
                        ih = 0
                        for d in range(2):
                            strict, tri = (k.m_gt, k.m_le) if d == 0 else (k.m_lt, k.m_ge)
                            order = [0, 1] + list(range(2, 18)) if d == 0 else [1, 0] + list(range(17, 1, -1))
                            P.memset("dve", H, H.ap, 0.0)
                            P.memset("dve", Hb, Hb.ap, 0.0)
                            bufs = {}

                            def s1a(c):
                                nonlocal it, ih
                                xdt, xw, cbm, E3, MT, to = xdts[it % 2], xws[it % 2], cbms[it % 2], E3s[it % 2], MTs[it % 2], tmpo[it % 2]
                                it += 1
                                lac = la.ap[:, c, d, :]
                                xsv = xs_tm.ap[:, c, :].rearrange("p (r q) -> p r q", q=64)
                                P.tt("dve", xdt.ap, xsv, dt.ap[:, c, d, :].unsqueeze(2).to_broadcast([128, 8, 64]), ALU.mult,
                                     [xs_tm, dt], [xdt])
                                pcb = P.psA()
                                P.mm(pcb, pcb.ap[:, 0:128], BT.ap[:, c * 128:(c + 1) * 128], CT.ap[:, c * 128:(c + 1) * 128],
                                     [BT, CT], True, True)
                                p3 = P.psC()
                                P.mm(p3, p3.ap[:, 0:8], tri.ap, lac, [tri, la], True, True)
                                P.mm(p3, p3.ap[:, 8:16], strict.ap, lac, [strict, la], True, True)
                                P.mm(p3, p3.ap[:, 16:24], k.onesF.ap, lac, [k.onesF, la], True, True)
                                P.act(E3.ap, p3.ap[:, 0:24], AF.Exp, [p3], [E3])
                                halves = []
                                for half in range(2):
                                    rD, ED = rDs[ih % 2], EDs[ih % 2]
                                    ih += 1
                                    P.tt("dve", rD.ap, tri.ap.unsqueeze(1).to_broadcast([128, 4, 128]),
                                         lac[:, half * 4:half * 4 + 4].unsqueeze(2).to_broadcast([128, 4, 128]), ALU.mult,
                                         [tri, la], [rD])
                                    pD = P.psA()
                                    P.mm(pD, pD.ap, strict.ap, rD.ap.rearrange("p r l -> p (r l)"), [strict, rD], True, True)
                                    P.act(ED.ap.rearrange("p r l -> p (r l)"), pD.ap, AF.Exp, [pD], [ED])
                                    halves.append(ED)
                                P.tt("dve", cbm.ap, pcb.ap[:, 0:128], tri.ap, ALU.mult, [pcb, tri], [cbm])
                                bufs[c] = (xdt, xw, cbm, E3, MT, to, halves)

                            def s1b(c):
                                xdt, xw, cbm, E3, MT, to, halves = bufs[c]
                                for half in range(2):
                                    P.tt("dve", MT.ap[:, half * 4:half * 4 + 4, :], halves[half].ap,
                                         cbm.ap.unsqueeze(1).to_broadcast([128, 4, 128]), ALU.mult, [halves[half], cbm], [MT])

                            def s2(c):
                                xdt, xw, cbm, E3, MT, to, halves = bufs.pop(c)
                                pY = P.psB()
                                for r in range(8):
                                    P.mm(pY, pY.ap[:, r * 64:(r + 1) * 64], MT.ap[:, r, :], xdt.ap[:, r, :], [MT, xdt], True, True)
                                pO = P.psB()
                                P.mm(pO, pO.ap, CT.ap[:, c * 128:(c + 1) * 128], Hb.ap, [CT, Hb], True, True)
                                P.tt("dve", xw.ap, xdt.ap, E3.ap[:, 8:16].unsqueeze(2).to_broadcast([128, 8, 64]), ALU.mult,
                                     [xdt, E3], [xw])
                                pS = P.psA()
                                P.mm(pS, pS.ap, Btm.ap[:, c, :], xw.ap.rearrange("p r q -> p (r q)"), [Btm, xw], True, True)
                                P.tt("dve", H.ap, H.ap, E3.ap[:, 16:24].unsqueeze(2).to_broadcast([128, 8, 64]), ALU.mult,
                                     [H, E3], [H])
                                P.tt("dve", H.ap, H.ap, pS.ap.rearrange("p (r q) -> p r q", q=64), ALU.add, [H, pS], [H])
                                P.copy("act", Hb.ap, H.ap.rearrange("p r q -> p (r q)"), [H], [Hb])
                                P.tt("dve", to.ap, pO.ap.rearrange("p (r q) -> p r q", q=64),
                                     E3.ap[:, 0:8].unsqueeze(2).to_broadcast([128, 8, 64]), ALU.mult, [pO, E3], [to])
                                yv = yacc.ap[:, c, :].rearrange("p (r q) -> p r q", q=64)
                                pYv = pY.ap.rearrange("p (r q) -> p r q", q=64)
                                if d == 0:
                                    P.tt("dve", yv, pYv, to.ap, ALU.add, [pY, to], [yacc])
                                else:
                                    P.tt("dve", to.ap, pYv, to.ap, ALU.add, [pY, to], [to])
                                    P.tt("dve", yv, yv, to.ap, ALU.add, [yacc, to], [yacc])

                            s1a(order[0])
                            s1b(order[0])
                            for oi, c in enumerate(order):
                                if oi + 1 < len(order):
                                    s1a(order[oi + 1])
                                s2(c)
                                if oi + 1 < len(order):
                                    s1b(order[oi + 1])
                            P.barrier()
                        ygs = [P.sb(eb, f"s_yg{j}", [128, 8, 64], F32) for j in range(2)]
                        ygb = [P.sb(eb, f"s_ygb{j}", [128, 512], BF16) for j in range(2)]
                        junk = P.sb(eb, "s_junk", [128, 512], F32)
                        col = P.sb(eb, "s_col", [128, 1], F32)
                        for c in range(18):
                            yg, yb_ = ygs[c % 2], ygb[c % 2]
                            xsv = xs_tm.ap[:, c, :].rearrange("p (r q) -> p r q", q=64)
                            P.tt("dve", yg.ap, xsv, rep.ap[:, 128 + g * 8:128 + (g + 1) * 8].unsqueeze(2).to_broadcast([128, 8, 64]),
                                 ALU.mult, [xs_tm, rep], [yg])
                            P.tt("dve", yg.ap, yg.ap, yacc.ap[:, c, :].rearrange("p (r q) -> p r q", q=64), ALU.add, [yg, yacc], [yg])
                            ygf = yg.ap.rearrange("p r q -> p (r q)")
                            P.tt("dve", ygf, ygf, zs.ap[:, c, :], ALU.mult, [yg, zs], [yg])
                            P.copy("act", yb_.ap, ygf, [yg], [yb_])
                            P.tt("dve", junk.ap, ygf, ygf, ALU.mult, [yg], [junk])
                            if g == 0:
                                P.op("dve", lambda e_: e_.reduce_sum(out=ssq.ap[:, b, c:c + 1], in_=junk.ap, axis=AX.X), [junk], [ssq])
                            else:
                                P.op("dve", lambda e_: e_.reduce_sum(out=col.ap, in_=junk.ap, axis=AX.X), [junk], [col])
                                P.tt("dve", ssq.ap[:, b, c:c + 1], ssq.ap[:, b, c:c + 1], col.ap, ALU.add, [ssq, col], [ssq])
                            P.dma("act", k.yS[b, c * 128:(c + 1) * 128, g * 512:(g + 1) * 512], yb_.ap, [yb_], [k.ySd[b][c]])
                    P.barrier()
        with ExitStack() as ef:
            wo = P.sb(ef, "s_wo", [128, 16, D], BF16)
            for kc in range(0, 16, 4):
                load_w_bf16(k, wo, wo.ap[:, kc:kc + 4, :],
                            k.w["ssm_w_out"][0, kc * 128:(kc + 4) * 128, :].rearrange("(c p) f -> p c f", p=128))
            rst = P.sb(ef, "s_rst", [128, NB, 18], F32)
            P.act(rst.ap, ssq.ap, AF.Sqrt, [ssq], [rst], bias=k.epsT.ap[:, 0:1], scale=1.0 / 2048)
            P.op("dve", lambda e_: e_.reciprocal(out=rst.ap, in_=rst.ap), [rst], [rst])
            yin = [P.sb(ef, f"s_yin{j}", [128, 2048], BF16) for j in range(2)]
            yn = [P.sb(ef, f"s_yn{j}", [128, 2048], BF16) for j in range(2)]
            yT = [P.sb(ef, f"s_yT{j}", [128, 16, 512], BF16) for j in range(2)]
            hb = [P.sb(ef, f"s_h{j}", [128, 8, 512], F32) for j in range(2)]
            ib = 0
            for it, (b, r, t0, n) in enumerate(token_tiles(with_ctx=not last)):
                yt, h = yT[it % 2], hb[it % 2]
                P.dma("sp", h.ap[:, :, 0:n], k.hT[b, :, t0:t0 + n].rearrange("(c p) t -> p c t", p=128), hdeps(k.hTd, b, t0, n), [h])
                for j in range(n // 128):
                    blk = t0 // 128 + j
                    yi, yo = yin[ib % 2], yn[ib % 2]
                    ib += 1
                    P.dma("sp", yi.ap, k.yS[b, blk * 128:(blk + 1) * 128, :], [k.ySd[b][blk]], [yi])
                    P.stt(yo.ap, yi.ap, rst.ap[:, b, blk:blk + 1], ngR.ap, ALU.mult, ALU.mult, [yi, rst, ngR], [yo])
                    for k8 in range(2):
                        ps = P.psB()
                        pv = ps.ap.bitcast(BF16)
                        for q in range(8):
                            kc = k8 * 8 + q
                            P.tr(ps, pv[:, q * 128:(q + 1) * 128], yo.ap[:, kc * 128:(kc + 1) * 128], k.identB, [yo], signal=(q == 7))
                        P.copy("act" if k8 == 0 else "dve", yt.ap[:, k8 * 8:(k8 + 1) * 8, j * 128:(j + 1) * 128],
                               pv.rearrange("p (q t) -> p q t", t=128), [ps], [yt])
                for oc in range(8):
                    ps = P.psA()
                    for kc in range(16):
                        P.mm(ps, ps.ap[:, 0:n], wo.ap[:, kc, oc * 128:(oc + 1) * 128], yt.ap[:, kc, 0:n], [wo, yt], kc == 0, kc == 15)
                    P.stt(h.ap[:, oc, 0:n], ps.ap[:, 0:n], k.modT.ap[:, i, 16 + oc, r:r + 1], h.ap[:, oc, 0:n], ALU.mult, ALU.add,
                          [ps, k.modT, h], [h])
                P.dma("act", k.hT[b, :, t0:t0 + n].rearrange("(c p) t -> p c t", p=128), h.ap[:, :, 0:n], [h], hdeps(k.hTd, b, t0, n))
    P.barrier()


def emit_all(k, es):
    P = k.P
    emit_consts(k, es)
    k.epsT = P.sb(es, "epsT", [128, 1], F32)
    P.memset("dve", k.epsT, k.epsT.ap, EPS)
    k.oneT = P.sb(es, "oneT", [128, 1], F32)
    P.memset("dve", k.oneT, k.oneT.ap, 1.0)
    emit_mod(k, es)
    emit_load(k)
    st = k.stages
    for i in range(4):
        last = i == 3
        if st is None or ("mix%d" % i) in st:
            emit_modulate(k, i, last)
            MIXERS[i](k, i, last)
        if st is None or ("ffn%d" % i) in st:
            emit_ffn(k, i, last, moe=(i % 2 == 1))
    emit_store(k)


def mixer_todo(k, i, last):
    pass


MIXERS = [mixer_ssd, mixer_gqa, mixer_pool, mixer_lru]


def rope_tables():
    rows = SEQ // 64
    row = np.repeat(np.arange(rows), 64).astype(np.float32)
    col = np.tile(np.arange(64), rows).astype(np.float32)
    nf = 16
    inv = (10000.0 ** (-np.arange(nf, dtype=np.float32) / nf)).astype(np.float32)
    ang = np.concatenate([row[:, None] * inv, col[:, None] * inv], axis=-1).astype(np.float32)
    cos = np.cos(ang).astype(np.float32).T
    sin = np.sin(ang).astype(np.float32).T
    return (np.ascontiguousarray(np.concatenate([cos, cos], 0)), np.ascontiguousarray(np.concatenate([sin, sin], 0)))


def make_in_maps(inputs, n_cores=8, dummy=()):
    cos, sin = rope_tables()
    maps = []
    for ci in range(n_cores):
        m = {"x": np.ascontiguousarray(inputs["x"][ci * NB:(ci + 1) * NB]),
             "c": np.ascontiguousarray(inputs["c"][ci * NB:(ci + 1) * NB]),
             "ctx": np.ascontiguousarray(inputs["ctx"][ci * NB:(ci + 1) * NB]),
             "rope_cos": cos, "rope_sin": sin}
        for name, _ in WEIGHT_SPECS:
            m[name] = np.zeros([128], np.float32) if name in dummy else np.ascontiguousarray(inputs[name], dtype=np.float32)
        maps.append(m)
    return maps


def kernel(**inputs):
    nc, _ = build()
    maps = make_in_maps(inputs, 8)
    res = run_bass_kernel_spmd(nc, maps, core_ids=list(range(8)))
    return np.concatenate([np.asarray(r["out"]) for r in res.results], axis=0).astype(np.float32)
```
